# Optimizing a Trainium2 kernel written in Bass

```python
import jax, jax.numpy as jnp
from jax import lax
import numpy as np


D_MODEL = 1024
BATCH = 16
SEQ = 2048
DEPTH = 2

CTX_LEN = 256
GRID_W = 64
N_EVEN = (DEPTH + 1) // 2
N_ODD = DEPTH // 2
RMS_EPS = 1e-6
ADA_STD = 0.5 * D_MODEL ** -0.5

A_HEADS = 4
A_DK = 128
A_DV = 128
A_CHUNK = 64
A_WIDTH = A_HEADS * A_DV
B_GROUPS = 4
B_CH = 128
B_CHUNK = 128
B_WIDTH = B_GROUPS * B_CH
MIX_WIDTH = A_WIDTH + B_WIDTH
EVEN_IN = 3 * A_HEADS * A_DK + 2 * A_WIDTH + 2 * B_WIDTH
C_HEADS = 8
C_NOPE = 128
C_ROPE = 64
C_V = 128
C_QRANK = 384
C_KVRANK = 256
C_IN = C_QRANK + C_KVRANK + C_ROPE
Q_BLOCK = 128
ROPE_THETA = 10000.0
ATTN_SCALE = (C_NOPE + C_ROPE) ** -0.5
M_GROUPS = 4
M_PER_GROUP = 8
M_EXPERTS = M_GROUPS * M_PER_GROUP
M_TOPK = 2
M_FF = 512
M_ROWS = 256

kernel_name = 'hybrid_hgrn2_gmlp_mla_hmoe_dit'


def rmsnorm(x, g):
    xf = x.astype(jnp.float32)
    y = xf * lax.rsqrt(jnp.mean(xf * xf, axis=-1, keepdims=True) + RMS_EPS)
    return (y * g.astype(jnp.float32)).astype(x.dtype)


def layernorm(x, g):
    xf = x.astype(jnp.float32)
    mu = jnp.mean(xf, axis=-1, keepdims=True)
    var = jnp.mean(jnp.square(xf - mu), axis=-1, keepdims=True)
    return ((xf - mu) * lax.rsqrt(var + RMS_EPS) * g.astype(jnp.float32)).astype(x.dtype)


def adaln(cond, w, b):
    return jnp.split(jax.nn.silu(cond) @ w + b, 6, axis=-1)


def modulate(h, shift, scale):
    return h * (1 + scale) + shift


def axial_angles(rows):
    half = C_ROPE // 2
    inv = ROPE_THETA ** (-jnp.arange(0, half, 2, dtype=jnp.float32) / half)
    r = jnp.repeat(jnp.arange(rows, dtype=jnp.float32), GRID_W)
    col = jnp.tile(jnp.arange(GRID_W, dtype=jnp.float32), rows)
    return r[:, None] * inv, col[:, None] * inv


def _rotate(x, ang):
    x1, x2 = jnp.split(x.astype(jnp.float32), 2, axis=-1)
    cos, sin = jnp.cos(ang), jnp.sin(ang)
    return jnp.concatenate([x1 * cos - x2 * sin, x1 * sin + x2 * cos], axis=-1)


def rope2d(x, ang_r, ang_c):
    half = C_ROPE // 2
    shape = (x.shape[1],) + (1,) * (x.ndim - 3) + (ang_r.shape[-1],)
    out = jnp.concatenate([_rotate(x[..., :half], ang_r.reshape(shape)),
                           _rotate(x[..., half:], ang_c.reshape(shape))], axis=-1)
    return out.astype(x.dtype)


def gla_chunk(q, k, v, logf, s0):
    bsz, t, h, _ = q.shape
    dv = v.shape[-1]
    n = t // A_CHUNK
    cf = lambda a: a.astype(jnp.float32).reshape(bsz, n, A_CHUNK, h, a.shape[-1])
    q, k, v, logf = cf(q), cf(k), cf(v), cf(logf)
    b = jnp.cumsum(logf, axis=2)
    b_last = b[:, :, -1]
    q_dec = q * jnp.exp(b)
    k_dec = k * jnp.exp(-b)
    k_tail = k * jnp.exp(b_last[:, :, None] - b)
    incl = jnp.tril(jnp.ones((A_CHUNK, A_CHUNK), dtype=bool))
    scores = jnp.where(incl, jnp.einsum('bnthk,bnshk->bnhts', q_dec, k_dec), 0.0)
    o_intra = jnp.einsum('bnhts,bnshv->bnthv', scores, v)
    ds = jnp.einsum('bnshk,bnshv->bnhkv', k_tail, v)
    decay = jnp.exp(b_last)

    def step(s, inp):
        d, u = inp
        return d[..., None] * s + u, s

    s_fin, s_in = lax.scan(step, s0.astype(jnp.float32),
                           (jnp.moveaxis(decay, 1, 0), jnp.moveaxis(ds, 1, 0)))
    s_in = jnp.moveaxis(s_in, 0, 1)
    o_inter = jnp.einsum('bnthk,bnhkv->bnthv', q_dec, s_in)
    return (o_intra + o_inter).reshape(bsz, t, h, dv), s_fin


def hgrn_out(o, g_raw, gain):
    o = rmsnorm(o, gain)
    g = g_raw.reshape(o.shape).astype(jnp.float32)
    return (o * jax.nn.silu(g)).reshape(o.shape[0], o.shape[1], A_WIDTH).astype(g_raw.dtype)


def spatial_gating(u, v, gain, w_s, b_s):
    u = jax.nn.gelu(u)
    v = layernorm(jax.nn.gelu(v), gain)
    bsz, t, _ = v.shape
    vb = v.reshape(bsz, t // B_CHUNK, B_CHUNK, B_GROUPS, B_CH)
    sv = jnp.einsum('gts,bnsgc->bntgc', w_s, vb) + b_s.T[:, :, None]
    return u * sv.reshape(bsz, t, B_WIDTH)


def even_mixer(hx, hc, w_in, lb, onorm_g, sgu_g, w_s, b_s, w_out, need_ctx):
    qk = A_HEADS * A_DK
    cuts = [qk, 2 * qk, 3 * qk, 3 * qk + A_WIDTH, 3 * qk + 2 * A_WIDTH, 3 * qk + 2 * A_WIDTH + B_WIDTH]
    px = jnp.split(hx @ w_in, cuts, axis=-1)
    pc = jnp.split(hc @ w_in, cuts, axis=-1)
    heads = lambda a: a.reshape(a.shape[0], a.shape[1], A_HEADS, -1)
    lb_h = lb.reshape(A_HEADS, A_DK)

    def gates(f_raw):
        f = lb_h + (1.0 - lb_h) * jax.nn.sigmoid(heads(f_raw).astype(jnp.float32))
        return 1.0 - f, jnp.log(f)

    qx, ix, qc, ic = heads(px[0]), heads(px[3]), heads(pc[0]), heads(pc[3])
    bsz = hx.shape[0]
    zeros = jnp.zeros((bsz, A_HEADS, A_DK, A_DV), jnp.float32)
    o_x = 0.0
    o_c = 0.0
    for f_idx, rev in ((1, False), (2, True)):
        tr = (lambda a: jnp.flip(a, axis=1)) if rev else (lambda a: a)
        kc, lfc = gates(pc[f_idx])
        kx, lfx = gates(px[f_idx])
        oc_d, s_ctx = gla_chunk(tr(qc), tr(kc), tr(ic), tr(lfc), zeros)
        ox_d, _ = gla_chunk(tr(qx), tr(kx), tr(ix), tr(lfx), s_ctx)
        o_c = o_c + tr(oc_d)
        o_x = o_x + tr(ox_d)
    y_x = jnp.concatenate([hgrn_out(o_x, px[4], onorm_g),
                           spatial_gating(px[5], px[6], sgu_g, w_s, b_s)], axis=-1) @ w_out
    if not need_ctx:
        return y_x, None
    y_c = jnp.concatenate([hgrn_out(o_c, pc[4], onorm_g),
                           spatial_gating(pc[5], pc[6], sgu_g, w_s, b_s)], axis=-1) @ w_out
    return y_x, y_c


def attend(qn, qr, kn, kr, v):
    s = (jnp.einsum('bqhd,bkhd->bhqk', qn, kn).astype(jnp.float32)
         + jnp.einsum('bqhr,bkr->bhqk', qr, kr).astype(jnp.float32))
    p = jax.nn.softmax(s * ATTN_SCALE, axis=-1)
    return jnp.einsum('bhqk,bkhv->bqhv', p.astype(v.dtype), v)


def mla_mixer(hx, hc, w_in, q_g, w_uq, kv_g, w_ukv, w_out, ang_r, ang_c, need_ctx):
    def q_side(p):
        q = (rmsnorm(p, q_g) @ w_uq).reshape(p.shape[0], p.shape[1], C_HEADS, C_NOPE + C_ROPE)
        return q[..., :C_NOPE], q[..., C_NOPE:]

    def kv_side(p):
        kv = (rmsnorm(p[..., :C_KVRANK], kv_g) @ w_ukv).reshape(p.shape[0], p.shape[1], C_HEADS, C_NOPE + C_V)
        return kv[..., :C_NOPE], p[..., C_KVRANK:], kv[..., C_NOPE:]

    bsz, t = hx.shape[0], hx.shape[1]
    px = hx @ w_in
    qn_x, qr_x = q_side(px[..., :C_QRANK])
    kn_x, kr_x, v_x = kv_side(px[..., C_QRANK:])
    kn_c, kr_c, v_c = kv_side(hc @ w_in[:, C_QRANK:])
    qr_x = rope2d(qr_x, ang_r, ang_c)
    kr_x = rope2d(kr_x, ang_r, ang_c)
    kn = jnp.concatenate([kn_c, kn_x], axis=1)
    kr = jnp.concatenate([kr_c, kr_x], axis=1)
    v = jnp.concatenate([v_c, v_x], axis=1)
    nb = t // Q_BLOCK
    blk = lambda a: jnp.swapaxes(a.reshape((bsz, nb, Q_BLOCK) + a.shape[2:]), 0, 1)
    o_x = lax.map(lambda qs: attend(qs[0], qs[1], kn, kr, v), (blk(qn_x), blk(qr_x)))
    y_x = jnp.swapaxes(o_x, 0, 1).reshape(bsz, t, C_HEADS * C_V) @ w_out
    if not need_ctx:
        return y_x, None
    qn_c, qr_c = q_side(hc @ w_in[:, :C_QRANK])
    o_c = attend(qn_c, qr_c, kn_c, kr_c, v_c)
    return y_x, o_c.reshape(bsz, hc.shape[1], C_HEADS * C_V) @ w_out


def hier_moe(h, w_r1, b_r1, w_r2, b_r2, w_gu, w_dn):
    n_tok, d = h.shape
    hf = h.astype(jnp.float32)
    p1 = jax.nn.softmax(hf @ w_r1.astype(jnp.float32) + b_r1, axis=-1)
    p_grp, grp = lax.top_k(p1, 1)
    logits2 = jnp.einsum('nd,gde->nge', hf, w_r2.astype(jnp.float32)) + b_r2
    logits2 = jnp.take_along_axis(logits2, grp[:, :, None], axis=1)[:, 0]
    p_top, e_top = lax.top_k(jax.nn.softmax(logits2, axis=-1), M_TOPK)
    weight = p_grp * p_top / jnp.sum(p_top, axis=-1, keepdims=True)
    assign = (grp * M_PER_GROUP + e_top).reshape(-1)
    n_assign = n_tok * M_TOPK
    order = jnp.argsort(assign)
    tok = order // M_TOPK
    e_sorted = assign[order]
    counts = jnp.zeros((M_EXPERTS,), jnp.int32).at[assign].add(1)
    padded = (counts + M_ROWS - 1) // M_ROWS * M_ROWS
    pad_end = jnp.cumsum(padded)
    pad_start = pad_end - padded
    start = jnp.cumsum(counts) - counts
    dest = pad_start[e_sorted] + jnp.arange(n_assign, dtype=jnp.int32) - start[e_sorted]
    n_rows = -(-(n_assign + M_EXPERTS * (M_ROWS - 1)) // M_ROWS) * M_ROWS
    n_blocks = n_rows // M_ROWS
    xs = jnp.zeros((n_rows, d), h.dtype).at[dest].set(h[tok])
    block_expert = jnp.minimum(
        jnp.searchsorted(pad_end, jnp.arange(n_blocks, dtype=jnp.int32) * M_ROWS, side='right'),
        M_EXPERTS - 1)

    def expert_block(args):
        xb, e = args
        g, u = jnp.split(xb @ w_gu[e], 2, axis=-1)
        return (jax.nn.silu(g) * u) @ w_dn[e]

    ys = lax.map(expert_block, (xs.reshape(n_blocks, M_ROWS, d), block_expert)).reshape(n_rows, d)
    y = ys[dest] * weight.reshape(-1)[order][:, None].astype(h.dtype)
    return jnp.zeros_like(h).at[tok].add(y)


def setup_inputs(seed: int = 0) -> dict:
    key = jax.random.key(seed)
    ks = iter(jax.random.split(key, 32))
    nrm = lambda shape, s: jax.random.normal(next(ks), shape, jnp.float32) * s
    gain = lambda shape: 1.0 + nrm(shape, 0.01)
    D = D_MODEL
    return {
        'x': nrm((BATCH, SEQ, D), 1.0),
        'c': nrm((BATCH, D), 1.0),
        'ctx': nrm((BATCH, CTX_LEN, D), 1.0),
        'c_ctx': nrm((D,), 1.0),
        'ada_w': nrm((DEPTH, D, 6 * D), ADA_STD),
        'ada_b': nrm((DEPTH, 6 * D), 0.01),
        'norm_mix_g': gain((DEPTH, D)),
        'norm_ffn_g': gain((DEPTH, D)),
        'ev_w_in': nrm((N_EVEN, D, EVEN_IN), D ** -0.5),
        'ev_lb_logits': nrm((N_EVEN + 1, A_HEADS * A_DK), 0.1),
        'ev_onorm_g': gain((N_EVEN, A_DV)),
        'ev_sgu_g': gain((N_EVEN, B_WIDTH)),
        'ev_w_s': nrm((N_EVEN, B_GROUPS, B_CHUNK, B_CHUNK), B_CHUNK ** -0.5),
        'ev_b_s': 1.0 + nrm((N_EVEN, B_GROUPS, B_CHUNK), 0.01),
        'ev_w_out': nrm((N_EVEN, MIX_WIDTH, D), MIX_WIDTH ** -0.5),
        'od_w_in': nrm((N_ODD, D, C_IN), D ** -0.5),
        'od_q_norm_g': gain((N_ODD, C_QRANK)),
        'od_w_uq': nrm((N_ODD, C_QRANK, C_HEADS * (C_NOPE + C_ROPE)), C_QRANK ** -0.5),
        'od_kv_norm_g': gain((N_ODD, C_KVRANK)),
        'od_w_ukv': nrm((N_ODD, C_KVRANK, C_HEADS * (C_NOPE + C_V)), C_KVRANK ** -0.5),
        'od_w_out': nrm((N_ODD, C_HEADS * C_V, D), (C_HEADS * C_V) ** -0.5),
        'moe_w_r1': nrm((DEPTH, D, M_GROUPS), D ** -0.5),
        'moe_b_r1': nrm((DEPTH, M_GROUPS), 0.01),
        'moe_w_r2': nrm((DEPTH, M_GROUPS, D, M_PER_GROUP), D ** -0.5),
        'moe_b_r2': nrm((DEPTH, M_GROUPS, M_PER_GROUP), 0.01),
        'moe_w_gu': nrm((DEPTH, M_EXPERTS, D, 2 * M_FF), D ** -0.5),
        'moe_w_dn': nrm((DEPTH, M_EXPERTS, M_FF, D), M_FF ** -0.5),
        'final_g': gain((D,)),
    }


def reference(x, c, ctx, c_ctx, ada_w, ada_b, norm_mix_g, norm_ffn_g,
              ev_w_in, ev_lb_logits, ev_onorm_g, ev_sgu_g, ev_w_s, ev_b_s, ev_w_out,
              od_w_in, od_q_norm_g, od_w_uq, od_kv_norm_g, od_w_ukv, od_w_out,
              moe_w_r1, moe_b_r1, moe_w_r2, moe_b_r2, moe_w_gu, moe_w_dn, final_g):
    bsz, n, d = x.shape
    rows = n // GRID_W
    ang_r, ang_c = axial_angles(rows)
    lb_all = jnp.cumsum(jax.nn.softmax(ev_lb_logits.astype(jnp.float32), axis=0), axis=0)
    for layer in range(DEPTH):
        last = layer == DEPTH - 1
        j = layer // 2
        mx = [m[:, None, :] for m in adaln(c, ada_w[layer], ada_b[layer])]
        mc = adaln(c_ctx, ada_w[layer], ada_b[layer])
        hx = modulate(rmsnorm(x, norm_mix_g[layer]), mx[0], mx[1])
        hc = modulate(rmsnorm(ctx, norm_mix_g[layer]), mc[0], mc[1])
        if layer % 2 == 0:
            ox, oc = even_mixer(hx, hc, ev_w_in[j], lb_all[j], ev_onorm_g[j], ev_sgu_g[j],
                                ev_w_s[j], ev_b_s[j], ev_w_out[j], not last)
        else:
            ox, oc = mla_mixer(hx, hc, od_w_in[j], od_q_norm_g[j], od_w_uq[j], od_kv_norm_g[j],
                               od_w_ukv[j], od_w_out[j], ang_r, ang_c, not last)
        x = x + mx[2] * ox
        moe_args = (moe_w_r1[layer], moe_b_r1[layer], moe_w_r2[layer], moe_b_r2[layer],
                    moe_w_gu[layer], moe_w_dn[layer])
        hx2 = modulate(rmsnorm(x, norm_ffn_g[layer]), mx[3], mx[4])
        if last:
            x = x + mx[5] * hier_moe(hx2.reshape(-1, d), *moe_args).reshape(bsz, n, d)
        else:
            ctx = ctx + mc[2] * oc
            hc2 = modulate(rmsnorm(ctx, norm_ffn_g[layer]), mc[3], mc[4])
            y = hier_moe(jnp.concatenate([hx2.reshape(-1, d), hc2.reshape(-1, d)], axis=0), *moe_args)
            x = x + mx[5] * y[:bsz * n].reshape(bsz, n, d)
            ctx = ctx + mc[5] * y[bsz * n:].reshape(bsz, ctx.shape[1], d)
    return rmsnorm(x, final_g)
```

```python
from contextlib import ExitStack
import numpy as np
import concourse.bass as bass
import concourse.mybir as mybir
from concourse.bass_utils import run_bass_kernel_spmd

F32 = mybir.dt.float32
BF16 = mybir.dt.bfloat16
I32 = mybir.dt.int32
AF = mybir.ActivationFunctionType
ALU = mybir.AluOpType
AX = mybir.AxisListType

NCORES = 8
D = 1024
SEQ = 2048
CTX = 256
BPC = 2
NEXP = 32
CAP = 768
FF = 512
EPS = 1e-6


class Buf:
    __slots__ = ("name", "w", "r", "slot", "slot_sw", "psum")

    def __init__(self, name):
        self.name = name
        self.psum = False
        self.w = None
        self.r = {}
        self.slot = None
        self.slot_sw = None


class SemSlot:
    __slots__ = ("sem", "cnt")

    def __init__(self, sem):
        self.sem = sem
        self.cnt = 0


class EngState:
    def __init__(self, name, obj, sem):
        self.name = name
        self.obj = obj
        self.sem = sem
        self.cnt = 0
        self.waited = {}


class Tile:
    def __init__(self, t, name):
        self.t = t
        self.buf = Buf(name)

    def __getitem__(self, idx):
        return self.t[idx]


class EngProxy:
    def __init__(self, P, st):
        self.P = P
        self.st = st

    def __getattr__(self, opname):
        P, st = self.P, self.st

        def call(*args, r=(), w=(), inc=True, **kw):
            return P.emit(st, lambda: getattr(st.obj, opname)(*args, **kw), r, w, inc)
        return call


def _bufs(xs):
    out = []
    for x in xs:
        if isinstance(x, Tile):
            out.append(x.buf)
        elif isinstance(x, Buf):
            out.append(x)
        else:
            raise TypeError(type(x))
    return out


class Prog:
    def __init__(self, nc, stack):
        self.nc = nc
        self.gstack = stack
        self.engs = {}
        for name, obj in (("pe", nc.tensor), ("act", nc.scalar), ("dve", nc.vector),
                          ("pool", nc.gpsimd), ("sp", nc.sync)):
            sem = stack.enter_context(nc.semaphore("sem_" + name))
            self.engs[name] = EngState(name, obj, sem)
        self.PE = EngProxy(self, self.engs["pe"])
        self.ACT = EngProxy(self, self.engs["act"])
        self.DVE = EngProxy(self, self.engs["dve"])
        self.POOL = EngProxy(self, self.engs["pool"])
        self.SP = EngProxy(self, self.engs["sp"])
        self.free_slots = []
        self.free_slots_sw = []
        self.all_slots = []
        self.phase_bufs = []
        self.uid = 0

    def name(self, base):
        self.uid += 1
        return "%s_%d" % (base, self.uid)

    def tile(self, stack, base, shape, dtype):
        nm = self.name(base)
        t = stack.enter_context(self.nc.sbuf_tensor(nm, list(shape), dtype))
        tl = Tile(t, nm)
        self.phase_bufs.append(tl.buf)
        return tl

    def psum(self, stack, base, shape, dtype):
        nm = self.name(base)
        t = stack.enter_context(self.nc.psum_tensor(nm, list(shape), dtype))
        tl = Tile(t, nm)
        tl.buf.psum = True
        self.phase_bufs.append(tl.buf)
        return tl

    def _slot(self, buf, sw=False):
        attr, free = ("slot_sw", self.free_slots_sw) if sw else ("slot", self.free_slots)
        if getattr(buf, attr) is None:
            if free:
                setattr(buf, attr, free.pop())
            else:
                sem = self.gstack.enter_context(self.nc.semaphore(self.name("dsem")))
                sl = SemSlot(sem)
                setattr(buf, attr, sl)
                self.all_slots.append(sl)
        return getattr(buf, attr)

    def _wait(self, eng, key, val):
        if isinstance(key, str):
            if key == eng.name and eng.name == "pe":
                return
            sem = self.engs[key].sem
        else:
            sem = key.sem
            val = max(val, key.cnt)
        if eng.waited.get(key, 0) < val:
            eng.obj.wait_ge(sem, val)
            eng.waited[key] = val

    def _deps(self, eng, r, w):
        need = {}

        def add(ev):
            if ev is None:
                return
            k, v = ev
            if need.get(k, 0) < v:
                need[k] = v
        for b in r:
            add(b.w)
            if b.psum:
                for k, v in b.r.items():
                    if k != eng.name:
                        add((k, v))
        for b in w:
            add(b.w)
            for k, v in b.r.items():
                add((k, v))
        for k, v in need.items():
            self._wait(eng, k, v)

    def emit(self, eng, fn, r, w, inc=True):
        r = _bufs(r)
        w = _bufs(w)
        self._deps(eng, r, w)
        inst = fn()
        if inc:
            inst.then_inc(eng.sem, 1)
            eng.cnt += 1
            c = eng.cnt
        else:
            c = eng.cnt + 1
        for b in r:
            if b.r.get(eng.name, 0) < c:
                b.r[eng.name] = c
        for b in w:
            b.w = (eng.name, c)
            b.r = {}
        return inst

    def dma(self, engp, out, in_, r=(), w=(), indirect=None, **kw):
        eng = engp.st
        r = _bufs(r)
        w = _bufs(w)
        self._deps(eng, r, w)
        sw = eng.name == "pool"
        slot = self._slot((w or r)[0], sw)
        if sw and slot.cnt > 0:
            self._wait(eng, slot, slot.cnt)
        if indirect is None:
            inst = eng.obj.dma_start(out=out, in_=in_, **kw)
        else:
            inst = eng.obj.indirect_dma_start(out=out, in_=in_, **indirect, **kw)
        inst.then_inc(slot.sem, 16)
        slot.cnt += 16
        for b in r:
            b.r[slot] = slot.cnt
        for b in w:
            b.w = (slot, slot.cnt)
            b.r = {}
        return inst

    def fresh_slots(self):
        self.free_slots = []
        self.free_slots_sw = []

    def barrier(self, final=False):
        for eng in self.engs.values():
            for other in self.engs.values():
                if other is not eng and other.cnt > 0:
                    self._wait(eng, other.name, other.cnt)
            for slot in self.all_slots:
                if slot.cnt > 0:
                    self._wait(eng, slot, slot.cnt)

    def end_phase(self):
        self.barrier()
        for b in self.phase_bufs:
            if b.slot is not None:
                self.free_slots.append(b.slot)
                b.slot = None
            if b.slot_sw is not None:
                self.free_slots_sw.append(b.slot_sw)
                b.slot_sw = None
        self.phase_bufs = []


class Pool:
    def __init__(self, P, stack, base, shape, dtype, n, psum=False):
        mk = P.psum if psum else P.tile
        self.tiles = [mk(stack, base, shape, dtype) for _ in range(n)]
        self.i = 0

    def next(self):
        t = self.tiles[self.i % len(self.tiles)]
        self.i += 1
        return t


def rms_rstd(P, x_ap, junk, ss, rstd, r, ncols=D):
    P.ACT.activation(out=junk[:, 0:ncols], in_=x_ap, func=AF.Square, r=r, w=[junk])
    P.DVE.tensor_reduce(out=ss[:, 0:1], in_=junk[:, 0:ncols], axis=AX.X, op=ALU.add, r=[junk], w=[ss])
    P.DVE.tensor_scalar(rstd[:, 0:1], ss[:, 0:1], 1.0 / ncols, EPS, op0=ALU.mult, op1=ALU.add, r=[ss], w=[rstd])
    P.ACT.activation(out=rstd[:, 0:1], in_=rstd[:, 0:1], func=AF.Ln, r=[rstd], w=[rstd])
    P.ACT.activation(out=rstd[:, 0:1], in_=rstd[:, 0:1], func=AF.Exp, scale=-0.5, r=[rstd], w=[rstd])


def phase_adaln(P, io):
    nc = P.nc
    with ExitStack() as st:
        ccT = P.tile(st, "ccT", [128, 8, 3], F32)
        sT = P.tile(st, "sT", [128, 8, 3], BF16)
        P.dma(P.SP, out=ccT[:], in_=io["ccT"][:, :, :], w=[ccT])
        P.ACT.activation(out=sT[:], in_=ccT[:], func=AF.Silu, r=[ccT], w=[sT])
        wpool = Pool(P, st, "adaw", [128, 8, 512], BF16, 3)
        pspool = Pool(P, st, "adaps", [128, 512], F32, 2, psum=True)
        for l in range(2):
            bias = P.tile(st, "adab", [3, 6 * D], F32)
            gm = P.tile(st, "gm", [3, D], F32)
            gf = P.tile(st, "gf", [3, D], F32)
            res = P.tile(st, "adares", [3, 6 * D], F32)
            P.dma(P.SP, out=bias[:], in_=io["ada_b"][l:l + 1, :].partition_broadcast(3), w=[bias])
            P.dma(P.SP, out=gm[:], in_=io["norm_mix_g"][l:l + 1, :].partition_broadcast(3), w=[gm])
            P.dma(P.SP, out=gf[:], in_=io["norm_ffn_g"][l:l + 1, :].partition_broadcast(3), w=[gf])
            for cc in range(12):
                wt = wpool.next()
                P.dma(P.POOL, out=wt[:], in_=io["ada_w"][l, :, cc * 512:(cc + 1) * 512].rearrange("(c p) n -> p c n", p=128), w=[wt])
                ps = pspool.next()
                for kc in range(8):
                    P.PE.matmul(ps[0:3, :], lhsT=sT[:, kc, :], rhs=wt[:, kc, :], start=(kc == 0), stop=(kc == 7),
                                r=[sT, wt], w=[ps], inc=(kc == 7))
                P.DVE.tensor_tensor(res[:, cc * 512:(cc + 1) * 512], ps[0:3, :], bias[:, cc * 512:(cc + 1) * 512], op=ALU.add,
                                    r=[ps, bias], w=[res])
            P.DVE.scalar_tensor_tensor(res[:, D:2 * D], in0=res[:, D:2 * D], scalar=1.0, in1=gm[:], op0=ALU.add, op1=ALU.mult,
                                       r=[res, gm], w=[res])
            P.DVE.scalar_tensor_tensor(res[:, 4 * D:5 * D], in0=res[:, 4 * D:5 * D], scalar=1.0, in1=gf[:], op0=ALU.add, op1=ALU.mult,
                                       r=[res, gf], w=[res])
            P.dma(P.SP, out=io["modv"][l].rearrange("j k n -> j (k n)"), in_=res[:], r=[res])
        P.end_phase()


def load_bcast(P, tile, src_row):
    P.dma(P.SP, out=tile[:], in_=src_row.partition_broadcast(128), w=[tile])


def phase_moe(P, io, l, tiles, final, dbg=None):
    dbg = dbg or {}
    nc = P.nc
    NT = len(tiles)
    NROWS = NEXP * CAP
    xs, ys = io["xs"], io["ys"]
    with ExitStack() as st0:
        idxg = P.tile(st0, "idxg", [128, NT, 2], I32)
        wgt = P.tile(st0, "wgt", [128, NT, 2], F32)
        identb = P.tile(st0, "identb", [128, 128], BF16)
        P.dma(P.POOL, out=identb[:], in_=io["ident"][:, :], w=[identb])

        with ExitStack() as st:
            identf = P.tile(st, "identf", [128, 128], F32)
            tri = P.tile(st, "tri", [128, 128], BF16)
            ones = P.tile(st, "ones", [128, 128], BF16)
            ecb = P.tile(st, "ecb", [128, NEXP], F32)
            trash = P.tile(st, "trash", [128, 1], F32)
            wr = P.tile(st, "wr", [128, 8, 36], F32)
            rb = P.tile(st, "rb", [128, 36], F32)
            base = P.tile(st, "base", [128, NEXP], F32)
            zrow = P.tile(st, "zrow", [128, D], F32)
            A2 = [P.tile(st, "A2", [128, D], F32) for _ in range(3)]
            B2 = [P.tile(st, "B2", [128, D], F32) for _ in range(3)]
            P.dma(P.SP, out=identf[:], in_=io["ident"][:, :], w=[identf])
            P.dma(P.POOL, out=tri[:], in_=io["tri"][:, :], w=[tri])
            P.dma(P.SP, out=ecb[:], in_=io["ecb"][:, :], w=[ecb])
            P.dma(P.SP, out=trash[:], in_=io["trash"][:, :], w=[trash])
            P.dma(P.SP, out=wr[:], in_=io["wr"][l].rearrange("(c p) n -> p c n", p=128), w=[wr])
            P.dma(P.SP, out=rb[:], in_=io["rb"][l:l + 1, :].partition_broadcast(128), w=[rb])
            P.DVE.memset(ones[:], 1.0, w=[ones])
            P.DVE.memset(base[:], 0.0, w=[base])
            P.DVE.memset(zrow[:], 0.0, w=[zrow])
            P.dma(P.SP, out=ys[NROWS:NROWS + 128, :], in_=zrow[:], r=[zrow])
            for j in range(3):
                load_bcast(P, A2[j], io["modv"][l, j, 4:5, :])
                load_bcast(P, B2[j], io["modv"][l, j, 3:4, :])

            xpool = Pool(P, st, "mx", [128, D], F32, 2)
            hpool = Pool(P, st, "mh", [128, D], F32, 2)
            hbpool = Pool(P, st, "mhb", [128, D], BF16, 3)
            hTpool = Pool(P, st, "mhT", [128, 8, 128], F32, 2)
            junk = P.tile(st, "junk", [128, D], F32)
            small = Pool(P, st, "msm", [128, 200], F32, 3)
            a_pool = Pool(P, st, "mA", [128, NEXP], BF16, 2)
            idxs_pool = Pool(P, st, "midx", [128, 2], I32, 3)
            psT = Pool(P, st, "mpsT", [128, 8, 128], F32, 2, psum=True)
            psL = Pool(P, st, "mpsL", [128, 512], F32, 2, psum=True)
            psR = Pool(P, st, "mpsR", [128, 512], F32, 2, psum=True)

            for t, (src, dst, j) in enumerate(tiles):
                xt = xpool.next()
                P.dma(P.SP, out=xt[:], in_=src, w=[xt])
                sm = small.next()
                ss, rstd = sm[:, 0:1], sm[:, 1:2]
                P.ACT.activation(out=junk[:], in_=xt[:], func=AF.Square, r=[xt], w=[junk])
                P.DVE.tensor_reduce(out=ss, in_=junk[:], axis=AX.X, op=ALU.add, r=[junk], w=[sm])
                P.DVE.tensor_scalar(rstd, ss, 1.0 / D, EPS, op0=ALU.mult, op1=ALU.add, r=[sm], w=[sm])
                P.ACT.activation(out=rstd, in_=rstd, func=AF.Ln, r=[sm], w=[sm])
                P.ACT.activation(out=rstd, in_=rstd, func=AF.Exp, scale=-0.5, r=[sm], w=[sm])
                h = hpool.next()
                P.DVE.scalar_tensor_tensor(h[:], in0=xt[:], scalar=rstd, in1=A2[j][:], op0=ALU.mult, op1=ALU.mult,
                                           r=[xt, sm, A2[j]], w=[h])
                P.DVE.tensor_tensor(h[:], h[:], B2[j][:], op=ALU.add, r=[h, B2[j]], w=[h])
                hb = hbpool.next()
                P.ACT.copy(out=hb[:], in_=h[:], r=[h], w=[hb])
                pt = psT.next()
                for kc in range(8):
                    P.PE.transpose(pt[:, kc, :], h[:, kc * 128:(kc + 1) * 128], identf[:], r=[h, identf], w=[pt], inc=(kc == 7))
                hT = hTpool.next()
                P.ACT.copy(out=hT[:, 0:4, :], in_=pt[:, 0:4, :], r=[pt], w=[hT])
                P.DVE.tensor_copy(hT[:, 4:8, :], pt[:, 4:8, :], r=[pt], w=[hT])
                pl = psL.next()
                for kc in range(8):
                    P.PE.matmul(pl[:, 0:36], lhsT=hT[:, kc, :], rhs=wr[:, kc, :], start=(kc == 0), stop=(kc == 7),
                                r=[hT, wr], w=[pl], inc=(kc == 7))
                Lb = sm[:, 8:44]
                P.DVE.tensor_tensor(Lb, pl[:, 0:36], rb[:], op=ALU.add, r=[pl, rb], w=[sm])
                m1, nm1, s1, pg = sm[:, 2:3], sm[:, 3:4], sm[:, 4:5], sm[:, 5:6]
                ohg, pen, e1 = sm[:, 44:48], sm[:, 48:52], sm[:, 52:56]
                P.DVE.tensor_reduce(out=m1, in_=Lb[:, 0:4], axis=AX.X, op=ALU.max, r=[sm], w=[sm])
                P.DVE.tensor_scalar(ohg, Lb[:, 0:4], m1, None, op0=ALU.is_equal, r=[sm], w=[sm])
                P.DVE.tensor_scalar(nm1, m1, -1.0, None, op0=ALU.mult, r=[sm], w=[sm])
                P.ACT.activation(out=e1, in_=Lb[:, 0:4], func=AF.Exp, bias=nm1, scale=1.0, r=[sm], w=[sm])
                P.DVE.tensor_reduce(out=s1, in_=e1, axis=AX.X, op=ALU.add, r=[sm], w=[sm])
                P.DVE.reciprocal(pg, s1, r=[sm], w=[sm])
                P.DVE.tensor_scalar(pen, ohg, 1e30, -1e30, op0=ALU.mult, op1=ALU.add, r=[sm], w=[sm])
                l2m = sm[:, 56:88]
                P.DVE.tensor_tensor(l2m.rearrange("p (g e) -> p g e", g=4), Lb[:, 4:36].rearrange("p (g e) -> p g e", g=4),
                                    pen.unsqueeze(2).to_broadcast([128, 4, 8]), op=ALU.add, r=[sm], w=[sm])
                top8 = sm[:, 88:96]
                P.DVE.max(out=top8, in_=l2m, r=[sm], w=[sm])
                oh1, oh2 = sm[:, 96:128], sm[:, 128:160]
                P.DVE.tensor_scalar(oh1, l2m, top8[:, 0:1], None, op0=ALU.is_equal, r=[sm], w=[sm])
                P.DVE.tensor_scalar(oh2, l2m, top8[:, 1:2], None, op0=ALU.is_equal, r=[sm], w=[sm])
                dd, ed = sm[:, 6:7], sm[:, 7:8]
                P.DVE.tensor_tensor(dd, top8[:, 1:2], top8[:, 0:1], op=ALU.subtract, r=[sm], w=[sm])
                P.ACT.activation(out=ed, in_=dd, func=AF.Exp, r=[sm], w=[sm])
                P.DVE.tensor_scalar(ed, ed, 1.0, None, op0=ALU.add, r=[sm], w=[sm])
                P.DVE.reciprocal(ed, ed, r=[sm], w=[sm])
                w1, w2 = sm[:, 2:3], sm[:, 3:4]
                P.DVE.tensor_tensor(w1, ed, pg, op=ALU.mult, r=[sm], w=[sm])
                P.DVE.tensor_tensor(w2, pg, w1, op=ALU.subtract, r=[sm], w=[sm])
                Ab = a_pool.next()
                P.DVE.tensor_tensor(Ab[:], oh1, oh2, op=ALU.add, r=[sm], w=[Ab])
                pr = psR.next()
                P.PE.matmul(pr[:, 0:32], lhsT=tri[:], rhs=Ab[:], start=True, stop=True, r=[tri, Ab], w=[pr])
                P.PE.matmul(pr[:, 32:64], lhsT=ones[:], rhs=Ab[:], start=True, stop=True, r=[ones, Ab], w=[pr])
                cum = sm[:, 44:76]
                P.DVE.tensor_tensor(cum, pr[:, 0:32], base[:], op=ALU.add, r=[pr, base], w=[sm])
                P.DVE.tensor_tensor(base[:], base[:], pr[:, 32:64], op=ALU.add, r=[pr, base], w=[base])
                val = sm[:, 8:40]
                P.DVE.tensor_scalar(val, cum, float(CAP), None, op0=ALU.is_le, r=[sm], w=[sm])
                P.DVE.tensor_tensor(cum, cum, ecb[:], op=ALU.add, r=[sm, ecb], w=[sm])
                prod = sm[:, 160:192]
                dv = sm[:, 192:196]
                for k, oh in enumerate((oh1, oh2)):
                    P.DVE.tensor_tensor(prod, oh, cum, op=ALU.mult, r=[sm], w=[sm])
                    P.DVE.tensor_reduce(out=dv[:, k:k + 1], in_=prod, axis=AX.X, op=ALU.add, r=[sm], w=[sm])
                    P.DVE.tensor_tensor(prod, oh, val, op=ALU.mult, r=[sm], w=[sm])
                    P.DVE.tensor_reduce(out=dv[:, 2 + k:3 + k], in_=prod, axis=AX.X, op=ALU.add, r=[sm], w=[sm])
                gi = sm[:, 196:198]
                si = sm[:, 198:200]
                nv = sm[:, 6:8]
                P.DVE.tensor_scalar(nv, dv[:, 2:4], -1.0, 1.0, op0=ALU.mult, op1=ALU.add, r=[sm], w=[sm])
                P.DVE.tensor_tensor(gi, dv[:, 0:2], dv[:, 2:4], op=ALU.mult, r=[sm], w=[sm])
                P.DVE.scalar_tensor_tensor(si, in0=nv, scalar=trash[:, 0:1], in1=gi, op0=ALU.mult, op1=ALU.add, r=[sm, trash], w=[sm])
                P.DVE.scalar_tensor_tensor(gi, in0=nv, scalar=float(NROWS), in1=gi, op0=ALU.mult, op1=ALU.add, r=[sm], w=[sm])
                P.DVE.tensor_tensor(wgt[:, t, :], sm[:, 2:4], dv[:, 2:4], op=ALU.mult, r=[sm], w=[wgt])
                P.DVE.tensor_copy(idxg[:, t, :], gi, r=[sm], w=[idxg])
                ix = idxs_pool.next()
                P.DVE.tensor_copy(ix[:], si, r=[sm], w=[ix])
                if "dbg_sm" in io:
                    P.dma(P.SP, out=io["dbg_sm"][t], in_=sm[:], r=[sm])
                for k in range(2):
                    if dbg.get("no_scatter"):
                        break
                    P.dma(P.POOL, out=xs[:, :], in_=hb[:], r=[hb, ix],
                          indirect=dict(out_offset=bass.IndirectOffsetOnAxis(ap=ix[:, k:k + 1], axis=0), in_offset=None))
            if "dbg_idx" in io:
                P.dma(P.SP, out=io["dbg_idx"][:, :, :], in_=idxg[:], r=[idxg])
                P.dma(P.SP, out=io["dbg_w"][:, :, :], in_=wgt[:], r=[wgt])
            P.end_phase()
        if dbg.get("moe_stop") == 1:
            return

        with ExitStack() as st:
            wgu_pool = Pool(P, st, "wgu", [128, 8, 2 * FF], BF16, 2)
            wdn_pool = Pool(P, st, "wdn", [128, 4, D], BF16, 2)
            RT = CAP // 128
            xsr_pool = Pool(P, st, "xsr", [128, RT, D], BF16, 2)
            xsT_pool = Pool(P, st, "xsT", [128, 8, CAP], BF16, 2)
            act_pool = Pool(P, st, "actT", [128, 4, CAP], BF16, 2)
            sg_pool = Pool(P, st, "sg", [128, 384], F32, 3)
            yo_pool = Pool(P, st, "yo", [128, D], F32, 3)
            psTb = Pool(P, st, "psTb", [128, 8, 128], BF16, 2, psum=True)
            psG = Pool(P, st, "psG", [128, 512], F32, 2, psum=True)
            psU = Pool(P, st, "psU", [128, 512], F32, 2, psum=True)
            psO = Pool(P, st, "psO", [128, 512], F32, 2, psum=True)
            NH = CAP // 384
            for e in range(NEXP):
                wgu = wgu_pool.next()
                wdn = wdn_pool.next()
                for hh in range(2):
                    P.dma(P.POOL, out=wgu[:, hh * 4:(hh + 1) * 4, :],
                          in_=io["moe_w_gu"][l, e, hh * 512:(hh + 1) * 512, :].rearrange("(c p) n -> p c n", p=128), w=[wgu])
                P.dma(P.POOL, out=wdn[:], in_=io["moe_w_dn"][l, e].rearrange("(c p) n -> p c n", p=128), w=[wdn])
                xsr = xsr_pool.next()
                P.dma(P.SP, out=xsr[:], in_=xs[e * CAP:(e + 1) * CAP, :].rearrange("(r p) k -> p r k", p=128), w=[xsr])
                xsT = xsT_pool.next()
                for rt in range(RT):
                    pt = psTb.next()
                    for kc in range(8):
                        P.PE.transpose(pt[:, kc, :], xsr[:, rt, kc * 128:(kc + 1) * 128], identb[:], r=[xsr, identb], w=[pt], inc=(kc == 7))
                    if rt % 2 == 0:
                        P.ACT.copy(out=xsT[:, :, rt * 128:(rt + 1) * 128], in_=pt[:], r=[pt], w=[xsT])
                    else:
                        P.DVE.tensor_copy(xsT[:, :, rt * 128:(rt + 1) * 128], pt[:], r=[pt], w=[xsT])
                actT = act_pool.next()
                for jf in range(4):
                    for nh in range(NH):
                        pg_, pu_ = psG.next(), psU.next()
                        for kc in range(8):
                            P.PE.matmul(pg_[:, 0:384], lhsT=wgu[:, kc, jf * 128:(jf + 1) * 128], rhs=xsT[:, kc, nh * 384:(nh + 1) * 384],
                                        start=(kc == 0), stop=(kc == 7), r=[wgu, xsT], w=[pg_], inc=(kc == 7))
                        for kc in range(8):
                            P.PE.matmul(pu_[:, 0:384], lhsT=wgu[:, kc, FF + jf * 128:FF + (jf + 1) * 128], rhs=xsT[:, kc, nh * 384:(nh + 1) * 384],
                                        start=(kc == 0), stop=(kc == 7), r=[wgu, xsT], w=[pu_], inc=(kc == 7))
                        sg = sg_pool.next()
                        P.ACT.activation(out=sg[:], in_=pg_[:, 0:384], func=AF.Silu, r=[pg_], w=[sg])
                        P.DVE.tensor_tensor(actT[:, jf, nh * 384:(nh + 1) * 384], sg[:], pu_[:, 0:384], op=ALU.mult, r=[sg, pu_], w=[actT])
                for rt in range(RT):
                    yo = yo_pool.next()
                    for n in range(2):
                        po = psO.next()
                        for fc in range(4):
                            P.PE.matmul(po[:], lhsT=actT[:, fc, rt * 128:(rt + 1) * 128], rhs=wdn[:, fc, n * 512:(n + 1) * 512],
                                        start=(fc == 0), stop=(fc == 3), r=[actT, wdn], w=[po], inc=(fc == 3))
                        if n == 0:
                            P.ACT.copy(out=yo[:, 0:512], in_=po[:], r=[po], w=[yo])
                        else:
                            P.DVE.tensor_copy(yo[:, 512:1024], po[:], r=[po], w=[yo])
                    P.dma(P.SP, out=ys[e * CAP + rt * 128:e * CAP + (rt + 1) * 128, :], in_=yo[:], r=[yo])
            P.end_phase()
        if dbg.get("moe_stop") == 2:
            return

        with ExitStack() as st:
            G2 = [P.tile(st, "G2", [128, D], F32) for _ in range(3)]
            for j in range(3):
                load_bcast(P, G2[j], io["modv"][l, j, 5:6, :])
            if final:
                fg = P.tile(st, "fg", [128, D], F32)
                load_bcast(P, fg, io["final_g"][0:1, :])
                junk = P.tile(st, "junk2", [128, D], F32)
            r1p = Pool(P, st, "r1", [128, D], F32, 3)
            r2p = Pool(P, st, "r2", [128, D], F32, 3)
            xp = Pool(P, st, "cx", [128, D], F32, 3)
            sp_ = Pool(P, st, "csm", [128, 4], F32, 3)
            for t, (src, dst, j) in enumerate(tiles):
                r1, r2, xt = r1p.next(), r2p.next(), xp.next()
                P.dma(P.POOL, out=r1[:], in_=ys[:, :], r=[idxg], w=[r1],
                      indirect=dict(out_offset=None, in_offset=bass.IndirectOffsetOnAxis(ap=idxg[:, t, 0:1], axis=0)))
                P.dma(P.POOL, out=r2[:], in_=ys[:, :], r=[idxg], w=[r2],
                      indirect=dict(out_offset=None, in_offset=bass.IndirectOffsetOnAxis(ap=idxg[:, t, 1:2], axis=0)))
                P.dma(P.SP, out=xt[:], in_=src, w=[xt])
                P.DVE.tensor_scalar(r1[:], r1[:], wgt[:, t, 0:1], None, op0=ALU.mult, r=[r1, wgt], w=[r1])
                P.DVE.scalar_tensor_tensor(r1[:], in0=r2[:], scalar=wgt[:, t, 1:2], in1=r1[:], op0=ALU.mult, op1=ALU.add,
                                           r=[r1, r2, wgt], w=[r1])
                P.DVE.tensor_tensor(r1[:], r1[:], G2[j][:], op=ALU.mult, r=[r1, G2[j]], w=[r1])
                P.DVE.tensor_tensor(xt[:], xt[:], r1[:], op=ALU.add, r=[xt, r1], w=[xt])
                if final:
                    sm = sp_.next()
                    P.ACT.activation(out=junk[:], in_=xt[:], func=AF.Square, r=[xt], w=[junk])
                    P.DVE.tensor_reduce(out=sm[:, 0:1], in_=junk[:], axis=AX.X, op=ALU.add, r=[junk], w=[sm])
                    P.DVE.tensor_scalar(sm[:, 1:2], sm[:, 0:1], 1.0 / D, EPS, op0=ALU.mult, op1=ALU.add, r=[sm], w=[sm])
                    P.ACT.activation(out=sm[:, 1:2], in_=sm[:, 1:2], func=AF.Ln, r=[sm], w=[sm])
                    P.ACT.activation(out=sm[:, 1:2], in_=sm[:, 1:2], func=AF.Exp, scale=-0.5, r=[sm], w=[sm])
                    P.DVE.scalar_tensor_tensor(xt[:], in0=xt[:], scalar=sm[:, 1:2], in1=fg[:], op0=ALU.mult, op1=ALU.mult,
                                               r=[xt, sm, fg], w=[xt])
                P.dma(P.SP, out=dst, in_=xt[:], r=[xt])
            P.end_phase()


def make_hT(P, pools, srcs, A, B, hT, identb, col0=0):
    for i, src in enumerate(srcs):
        xt = pools["x"].next()
        P.dma(P.SP, out=xt[:], in_=src, w=[xt])
        sm = pools["sm"].next()
        junk = pools["junk"]
        P.ACT.activation(out=junk[:], in_=xt[:], func=AF.Square, r=[xt], w=[junk])
        P.DVE.tensor_reduce(out=sm[:, 0:1], in_=junk[:], axis=AX.X, op=ALU.add, r=[junk], w=[sm])
        P.DVE.tensor_scalar(sm[:, 1:2], sm[:, 0:1], 1.0 / D, EPS, op0=ALU.mult, op1=ALU.add, r=[sm], w=[sm])
        P.ACT.activation(out=sm[:, 1:2], in_=sm[:, 1:2], func=AF.Ln, r=[sm], w=[sm])
        P.ACT.activation(out=sm[:, 1:2], in_=sm[:, 1:2], func=AF.Exp, scale=-0.5, r=[sm], w=[sm])
        h = pools["h"].next()
        P.DVE.scalar_tensor_tensor(h[:], in0=xt[:], scalar=sm[:, 1:2], in1=A[:], op0=ALU.mult, op1=ALU.mult, r=[xt, sm, A], w=[h])
        hb = pools["hb"].next()
        P.DVE.tensor_tensor(hb[:], h[:], B[:], op=ALU.add, r=[h, B], w=[hb])
        pt = pools["psT"].next()
        for kc in range(8):
            P.PE.transpose(pt[:, kc, :], hb[:, kc * 128:(kc + 1) * 128], identb[:], r=[hb, identb], w=[pt], inc=(kc == 7))
        c = col0 + i * 128
        if i % 2 == 0:
            P.ACT.copy(out=hT[:, :, c:c + 128], in_=pt[:], r=[pt], w=[hT])
        else:
            P.DVE.tensor_copy(hT[:, :, c:c + 128], pt[:], r=[pt], w=[hT])


def hT_pools(P, st):
    return dict(x=Pool(P, st, "nx", [128, D], F32, 2), sm=Pool(P, st, "nsm", [128, 4], F32, 3),
                junk=P.tile(st, "njunk", [128, D], F32), h=Pool(P, st, "nh", [128, D], F32, 1),
                hb=Pool(P, st, "nhb", [128, D], BF16, 1), psT=Pool(P, st, "npsT", [128, 8, 128], BF16, 1, psum=True))


def phase_mla(P, io, l, b, dbg=None):
    dbg = dbg or {}
    nc = P.nc
    T = CTX + SEQ
    NKT = T // 128
    SCALE = float((128 + 64) ** -0.5)
    xn_, cn_ = dbg.get("mix1_src", ("xres", "cres"))
    xrows = lambda i: io[xn_][b * SEQ + i * 128: b * SEQ + (i + 1) * 128, :]
    crows = lambda i: io[cn_][b * CTX + i * 128: b * CTX + (i + 1) * 128, :]
    with ExitStack() as st0:
        Kn = P.tile(st0, "Kn", [128, 8, T], BF16)
        Kr = P.tile(st0, "Kr", [128, T], BF16)
        V = P.tile(st0, "V", [128, NKT, 8, 129], BF16)
        qnT = P.tile(st0, "qnT", [128, 3, SEQ], BF16)
        identb = P.tile(st0, "identb", [128, 128], BF16)
        COS = P.tile(st0, "COS", [128, T], F32)
        SIN = P.tile(st0, "SIN", [128, T], F32)
        P.dma(P.POOL, out=identb[:], in_=io["ident"][:, :], w=[identb])
        P.dma(P.SP, out=COS[:], in_=io["ropecos"][:, :], w=[COS])
        P.dma(P.SP, out=SIN[:], in_=io["ropesin"][:, :], w=[SIN])
        P.DVE.memset(V[:, :, :, 128:129], 1.0, w=[V])
        with ExitStack() as st:
            win = P.tile(st, "win", [128, 8, 896], BF16)
            wukv = P.tile(st, "wukv", [128, 2, 2048], BF16)
            ones = P.tile(st, "ones", [128, 128], BF16)
            gq = P.tile(st, "gq", [128, 3], F32)
            gkv = P.tile(st, "gkv", [128, 2], F32)
            A1 = [P.tile(st, "A1", [128, D], F32) for _ in range(2)]
            B1 = [P.tile(st, "B1", [128, D], F32) for _ in range(2)]
            for hh in range(2):
                P.dma(P.POOL, out=win[:, hh * 4:(hh + 1) * 4, :], in_=io["od_w_in_ext"][hh * 512:(hh + 1) * 512, :].rearrange("(c p) n -> p c n", p=128), w=[win])
            for hh in range(2):
                P.dma(P.POOL, out=wukv[:, :, hh * 1024:(hh + 1) * 1024], in_=io["od_w_ukv_r"][:, hh * 1024:(hh + 1) * 1024].rearrange("(c p) n -> p c n", p=128), w=[wukv])
            P.dma(P.SP, out=gq[:], in_=io["od_gq"][:, :], w=[gq])
            P.dma(P.SP, out=gkv[:], in_=io["od_gkv"][:, :], w=[gkv])
            P.DVE.memset(ones[:], 1.0, w=[ones])
            for k_, j in enumerate((b, 2)):
                load_bcast(P, A1[k_], io["modv"][l, j, 1:2, :])
                load_bcast(P, B1[k_], io["modv"][l, j, 0:1, :])
            pools = hT_pools(P, st)
            hTp = Pool(P, st, "hT", [128, 8, 512], BF16, 1)
            cf = Pool(P, st, "cf", [128, 3, 512], F32, 1)
            sq = Pool(P, st, "sq", [128, 3, 512], BF16, 1)
            rs = Pool(P, st, "rs", [128, 512], F32, 2)
            kvn = Pool(P, st, "kvn", [128, 2, 512], BF16, 2)
            tmpf = Pool(P, st, "tmpf", [128, 512], F32, 2)
            psA = Pool(P, st, "psA", [128, 512], F32, 4, psum=True)
            psS = Pool(P, st, "psS", [128, 512], F32, 2, psum=True)

            def rms_feat(nch, w0, gain, ntok, hT, outT, oc0, nfeat):
                c_, s_ = cf.next(), sq.next()
                for fc in range(nch):
                    ps = psA.next()
                    for kc in range(8):
                        P.PE.matmul(ps[:, 0:ntok], lhsT=win[:, kc, w0 + fc * 128: w0 + (fc + 1) * 128], rhs=hT[:, kc, 0:ntok],
                                    start=(kc == 0), stop=(kc == 7), r=[win, hT], w=[ps], inc=(kc == 7))
                    P.DVE.tensor_copy(c_[:, fc, 0:ntok], ps[:, 0:ntok], r=[ps], w=[c_])
                    P.ACT.activation(out=s_[:, fc, 0:ntok], in_=c_[:, fc, 0:ntok], func=AF.Square, r=[c_], w=[s_])
                pss = psS.next()
                for fc in range(nch):
                    P.PE.matmul(pss[:, 0:ntok], lhsT=ones[:], rhs=s_[:, fc, 0:ntok], start=(fc == 0), stop=(fc == nch - 1),
                                r=[ones, s_], w=[pss], inc=(fc == nch - 1))
                r_ = rs.next()
                P.DVE.tensor_scalar(r_[:, 0:ntok], pss[:, 0:ntok], 1.0 / nfeat, EPS, op0=ALU.mult, op1=ALU.add, r=[pss], w=[r_])
                P.ACT.activation(out=r_[:, 0:ntok], in_=r_[:, 0:ntok], func=AF.Ln, r=[r_], w=[r_])
                P.ACT.activation(out=r_[:, 0:ntok], in_=r_[:, 0:ntok], func=AF.Exp, scale=-0.5, r=[r_], w=[r_])
                for fc in range(nch):
                    P.DVE.scalar_tensor_tensor(outT[:, fc, oc0:oc0 + ntok], in0=c_[:, fc, 0:ntok], scalar=gain[:, fc:fc + 1], in1=r_[:, 0:ntok],
                                               op0=ALU.mult, op1=ALU.mult, r=[c_, gain, r_], w=[outT])

            blocks = [(0, 256, [crows(i) for i in range(2)], 1, None)]
            for qb in range(4):
                blocks.append((256 + qb * 512, 512, [xrows(qb * 4 + i) for i in range(4)], 0, qb * 512))
            for (t0, ntok, srcs, ab, q0) in blocks:
                hT = hTp.next()
                make_hT(P, pools, srcs, A1[ab], B1[ab], hT, identb)
                kv_ = kvn.next()
                rms_feat(2, 384, gkv, ntok, hT, kv_, 0, 256)
                if q0 is not None:
                    rms_feat(3, 0, gq, ntok, hT, qnT, q0, 384)
                for hd in range(8):
                    ps = psA.next()
                    for kc in range(2):
                        P.PE.matmul(ps[:, 0:ntok], lhsT=wukv[:, kc, hd * 128:(hd + 1) * 128], rhs=kv_[:, kc, 0:ntok],
                                    start=(kc == 0), stop=(kc == 1), r=[wukv, kv_], w=[ps], inc=(kc == 1))
                    if hd % 2 == 0:
                        P.ACT.copy(out=Kn[:, hd, t0:t0 + ntok], in_=ps[:, 0:ntok], r=[ps], w=[Kn])
                    else:
                        P.DVE.tensor_copy(Kn[:, hd, t0:t0 + ntok], ps[:, 0:ntok], r=[ps], w=[Kn])
                for i in range(ntok // 128):
                    kt = t0 // 128 + i
                    for half in range(2):
                        ps = psA.next()
                        for kc in range(2):
                            P.PE.matmul(ps[:], lhsT=kv_[:, kc, i * 128:(i + 1) * 128], rhs=wukv[:, kc, 1024 + half * 512: 1024 + (half + 1) * 512],
                                        start=(kc == 0), stop=(kc == 1), r=[wukv, kv_], w=[ps], inc=(kc == 1))
                        if half == 0:
                            P.ACT.copy(out=V[:, kt, 0:4, 0:128], in_=ps[:].rearrange("p (h v) -> p h v", h=4), r=[ps], w=[V])
                        else:
                            P.DVE.tensor_copy(V[:, kt, 4:8, 0:128], ps[:].rearrange("p (h v) -> p h v", h=4), r=[ps], w=[V])
                pa, pb = psA.next(), psA.next()
                for kc in range(8):
                    P.PE.matmul(pa[:, 0:ntok], lhsT=win[:, kc, 640:768], rhs=hT[:, kc, 0:ntok], start=(kc == 0), stop=(kc == 7),
                                r=[win, hT], w=[pa], inc=(kc == 7))
                for kc in range(8):
                    P.PE.matmul(pb[:, 0:ntok], lhsT=win[:, kc, 768:896], rhs=hT[:, kc, 0:ntok], start=(kc == 0), stop=(kc == 7),
                                r=[win, hT], w=[pb], inc=(kc == 7))
                ta, tb = tmpf.next(), tmpf.next()
                P.DVE.tensor_tensor(ta[:, 0:ntok], pa[:, 0:ntok], COS[:, t0:t0 + ntok], op=ALU.mult, r=[pa, COS], w=[ta])
                P.DVE.tensor_tensor(tb[:, 0:ntok], pb[:, 0:ntok], SIN[:, t0:t0 + ntok], op=ALU.mult, r=[pb, SIN], w=[tb])
                P.DVE.tensor_tensor(Kr[:, t0:t0 + ntok], ta[:, 0:ntok], tb[:, 0:ntok], op=ALU.add, r=[ta, tb], w=[Kr])
            P.end_phase()
        if dbg.get("mla_stop") == "A":
            return

        with ExitStack() as st:
            wuq = P.tile(st, "wuq", [128, 3, 2048], BF16)
            wout = P.tile(st, "wout", [128, 8, D], BF16)
            G1 = P.tile(st, "G1", [128, D], F32)
            for hh in range(2):
                P.dma(P.POOL, out=wuq[:, :, hh * 1024:(hh + 1) * 1024], in_=io["od_w_uq_ext"][:, hh * 1024:(hh + 1) * 1024].rearrange("(c p) n -> p c n", p=128), w=[wuq])
            for hh in range(2):
                P.dma(P.POOL, out=wout[:, hh * 4:(hh + 1) * 4, :], in_=io["od_w_out"][hh * 512:(hh + 1) * 512, :].rearrange("(c p) n -> p c n", p=128), w=[wout])
            load_bcast(P, G1, io["modv"][l, b, 2:3, :])
            Qn = Pool(P, st, "Qn", [128, 8, 512], BF16, 2)
            Qr = Pool(P, st, "Qr", [128, 4, 512], BF16, 2)
            PT = Pool(P, st, "PT", [128, 512], BF16, 4)
            osb = Pool(P, st, "osb", [128, 4, D], BF16, 2)
            oTp = Pool(P, st, "oT", [128, 8, 128], BF16, 2)
            xp = Pool(P, st, "ax", [128, D], F32, 2)
            yp = Pool(P, st, "ay", [128, D], F32, 2)
            tmpf = Pool(P, st, "atmp", [128, 512], F32, 2)
            rinv = Pool(P, st, "rinv", [128, 4], F32, 4)
            psQ = Pool(P, st, "psQ", [128, 512], F32, 2, psum=True)
            psSc = Pool(P, st, "psSc", [128, 512], F32, 2, psum=True)
            psO = Pool(P, st, "psO", [128, 2, 256], F32, 2, psum=True)
            psT = Pool(P, st, "psTa", [128, 8, 128], BF16, 1, psum=True)
            for qb in range(4):
                q0 = qb * 512
                qn_, qr_ = Qn.next(), Qr.next()
                for hd in range(8):
                    ps = psQ.next()
                    for kc in range(3):
                        P.PE.matmul(ps[:], lhsT=wuq[:, kc, hd * 128:(hd + 1) * 128], rhs=qnT[:, kc, q0:q0 + 512], start=(kc == 0), stop=(kc == 2),
                                    r=[wuq, qnT], w=[ps], inc=(kc == 2))
                    if hd % 2 == 0:
                        P.ACT.copy(out=qn_[:, hd, :], in_=ps[:], r=[ps], w=[qn_])
                    else:
                        P.DVE.tensor_copy(qn_[:, hd, :], ps[:], r=[ps], w=[qn_])
                for pr in range(4):
                    pa, pb = psQ.next(), psQ.next()
                    for kc in range(3):
                        P.PE.matmul(pa[:], lhsT=wuq[:, kc, 1024 + pr * 128: 1024 + (pr + 1) * 128], rhs=qnT[:, kc, q0:q0 + 512], start=(kc == 0), stop=(kc == 2),
                                    r=[wuq, qnT], w=[pa], inc=(kc == 2))
                    for kc in range(3):
                        P.PE.matmul(pb[:], lhsT=wuq[:, kc, 1536 + pr * 128: 1536 + (pr + 1) * 128], rhs=qnT[:, kc, q0:q0 + 512], start=(kc == 0), stop=(kc == 2),
                                    r=[wuq, qnT], w=[pb], inc=(kc == 2))
                    ta, tb = tmpf.next(), tmpf.next()
                    P.DVE.tensor_tensor(ta[:], pa[:], COS[:, CTX + q0: CTX + q0 + 512], op=ALU.mult, r=[pa, COS], w=[ta])
                    P.DVE.tensor_tensor(tb[:], pb[:], SIN[:, CTX + q0: CTX + q0 + 512], op=ALU.mult, r=[pb, SIN], w=[tb])
                    P.DVE.tensor_tensor(qr_[:, pr, :], ta[:], tb[:], op=ALU.add, r=[ta, tb], w=[qr_])
                o_ = osb.next()
                for hd in range(8):
                    po = [psO.next(), psO.next()]
                    ph = (hd % 2) * 64
                    for kt in range(NKT):
                        sc = psSc.next()
                        P.PE.matmul(sc[:], lhsT=Kn[:, hd, kt * 128:(kt + 1) * 128], rhs=qn_[:, hd, :], start=True, stop=False,
                                    r=[Kn, qn_], w=[sc], inc=False)
                        P.PE.matmul(sc[:], lhsT=Kr[ph:ph + 64, kt * 128:(kt + 1) * 128], rhs=qr_[ph:ph + 64, hd // 2, :], start=False, stop=True,
                                    r=[Kr, qr_], w=[sc])
                        pt_ = PT.next()
                        P.ACT.activation(out=pt_[:], in_=sc[:], func=AF.Exp, scale=SCALE, r=[sc], w=[pt_])
                        for qi in range(4):
                            P.PE.matmul(po[qi // 2][:, qi % 2, 0:129], lhsT=pt_[:, qi * 128:(qi + 1) * 128], rhs=V[:, kt, hd, :],
                                        start=(kt == 0 and qi % 2 == 0), stop=(kt == NKT - 1), r=[pt_, V], w=[po[qi // 2]], inc=(qi == 3), skip_group_check=True)
                    ri = rinv.next()
                    for qi in range(4):
                        P.DVE.reciprocal(ri[:, qi:qi + 1], po[qi // 2][:, qi % 2, 128:129], r=[po[qi // 2]], w=[ri])
                    for qi in range(4):
                        P.DVE.tensor_scalar(o_[:, qi, hd * 128:(hd + 1) * 128], po[qi // 2][:, qi % 2, 0:128], ri[:, qi:qi + 1], None, op0=ALU.mult,
                                            r=[po[qi // 2], ri], w=[o_])
                for qi in range(4):
                    pt = psT.next()
                    for kc in range(8):
                        P.PE.transpose(pt[:, kc, :], o_[:, qi, kc * 128:(kc + 1) * 128], identb[:], r=[o_, identb], w=[pt], inc=(kc == 7))
                    oT = oTp.next()
                    P.ACT.copy(out=oT[:], in_=pt[:], r=[pt], w=[oT])
                    xt = xp.next()
                    P.dma(P.SP, out=xt[:], in_=xrows(qb * 4 + qi), w=[xt])
                    y = yp.next()
                    for n in range(2):
                        ps = psQ.next()
                        for kc in range(8):
                            P.PE.matmul(ps[:], lhsT=oT[:, kc, :], rhs=wout[:, kc, n * 512:(n + 1) * 512], start=(kc == 0), stop=(kc == 7),
                                        r=[oT, wout], w=[ps], inc=(kc == 7))
                        P.DVE.tensor_tensor(y[:, n * 512:(n + 1) * 512], ps[:], G1[:, n * 512:(n + 1) * 512], op=ALU.mult, r=[ps, G1], w=[y])
                    P.DVE.tensor_tensor(xt[:], xt[:], y[:], op=ALU.add, r=[xt, y], w=[xt])
                    P.dma(P.SP, out=xrows(qb * 4 + qi), in_=xt[:], r=[xt])
            P.end_phase()


def phase_mix0(P, io, l, b, dbg=None):
    dbg = dbg or {}
    T = CTX + SEQ
    NCH = T // 128
    xin = lambda i: io["x"][b * SEQ + i * 128: b * SEQ + (i + 1) * 128, :]
    cin = lambda i: io["ctx"][b * CTX + i * 128: b * CTX + (i + 1) * 128, :]
    xout = lambda i: io["xres"][b * SEQ + i * 128: b * SEQ + (i + 1) * 128, :]
    cout = lambda i: io["cres"][b * CTX + i * 128: b * CTX + (i + 1) * 128, :]
    fm, iv, sgd, mixcat = io["fm"], io["iv"], io["sgd"], io["mixcat"]
    with ExitStack() as st0:
        identb = P.tile(st0, "identb", [128, 128], BF16)
        P.dma(P.POOL, out=identb[:], in_=io["ident"][:, :], w=[identb])
        with ExitStack() as st:
            win = P.tile(st, "win0", [128, 8, 3584], BF16)
            for kc in range(8):
                for hh in range(2):
                    P.dma(P.POOL, out=win[:, kc, hh * 1792:(hh + 1) * 1792], in_=io["ev_w_in"][kc * 128:(kc + 1) * 128, hh * 1792:(hh + 1) * 1792], w=[win])
            wsT = P.tile(st, "wsT", [128, 4, 128], BF16)
            P.dma(P.POOL, out=wsT[:], in_=io["ev_wsT"].rearrange("g s t -> s g t"), w=[wsT])
            bsb = P.tile(st, "bsb", [128, 512], F32)
            P.dma(P.SP, out=bsb[:], in_=io["ev_bsb"][:, :], w=[bsb])
            sgg = P.tile(st, "sgg", [128, 512], F32)
            load_bcast(P, sgg, io["ev_sgu_g"][0:1, :])
            A1 = [P.tile(st, "A1", [128, D], F32) for _ in range(2)]
            B1 = [P.tile(st, "B1", [128, D], F32) for _ in range(2)]
            for k_, j in enumerate((b, 2)):
                load_bcast(P, A1[k_], io["modv"][l, j, 1:2, :])
                load_bcast(P, B1[k_], io["modv"][l, j, 0:1, :])
            pools = hT_pools(P, st)
            hTp = Pool(P, st, "hT0", [128, 8, 512], BF16, 1)
            fmt = Pool(P, st, "fmt", [128, 512], F32, 3)
            ivt = Pool(P, st, "ivt", [128, 512], BF16, 2)
            sgt = Pool(P, st, "sgt", [128, 512], BF16, 2)
            et = Pool(P, st, "et", [128, 512], F32, 2)
            gu = Pool(P, st, "gu", [128, 512], F32, 2)
            gv = Pool(P, st, "gv", [128, 512], F32, 2)
            vn = Pool(P, st, "vn", [128, 512], BF16, 2)
            so = Pool(P, st, "so", [128, 512], BF16, 2)
            sm4 = Pool(P, st, "sm4", [128, 8], F32, 3)
            junk = pools["junk"]
            psF = Pool(P, st, "psF", [128, 512], F32, 2, psum=True)
            psTk = Pool(P, st, "psTk", [128, 512], F32, 4, psum=True)
            psSp = Pool(P, st, "psSp", [128, 512], F32, 1, psum=True)
            blocks = [(0, 256, [cin(i) for i in range(2)], 1)]
            for qb in range(4):
                blocks.append((256 + qb * 512, 512, [xin(qb * 4 + i) for i in range(4)], 0))
            for (t0, ntok, srcs, ab) in blocks:
                hT = hTp.next()
                make_hT(P, pools, srcs, A1[ab], B1[ab], hT, identb)
                for c in range(12):
                    ps = psF.next()
                    for kc in range(8):
                        P.PE.matmul(ps[:, 0:ntok], lhsT=win[:, kc, c * 128:(c + 1) * 128], rhs=hT[:, kc, 0:ntok], start=(kc == 0), stop=(kc == 7),
                                    r=[win, hT], w=[ps], inc=(kc == 7))
                    f_ = fmt.next()
                    if c % 2 == 0:
                        P.ACT.copy(out=f_[:, 0:ntok], in_=ps[:, 0:ntok], r=[ps], w=[f_])
                    else:
                        P.DVE.tensor_copy(f_[:, 0:ntok], ps[:, 0:ntok], r=[ps], w=[f_])
                    P.dma(P.SP, out=fm[c, :, t0:t0 + ntok], in_=f_[:, 0:ntok], r=[f_])
                for i in range(ntok // 128):
                    r0 = t0 + i * 128
                    pk = [psTk.next() for _ in range(4)]
                    for q_ in range(4):
                        for kc in range(8):
                            P.PE.matmul(pk[q_][:], lhsT=hT[:, kc, i * 128:(i + 1) * 128], rhs=win[:, kc, 1536 + q_ * 512: 1536 + (q_ + 1) * 512],
                                        start=(kc == 0), stop=(kc == 7), r=[win, hT], w=[pk[q_]], inc=(kc == 7))
                    iv_ = ivt.next()
                    P.ACT.copy(out=iv_[:], in_=pk[0][:], r=[pk[0]], w=[iv_])
                    P.dma(P.SP, out=iv[r0:r0 + 128, :], in_=iv_[:], r=[iv_])
                    e_ = et.next()
                    P.ACT.activation(out=e_[:], in_=pk[1][:], func=AF.Exp, scale=-1.0, r=[pk[1]], w=[e_])
                    P.DVE.tensor_scalar(e_[:], e_[:], 1.0, None, op0=ALU.add, r=[e_], w=[e_])
                    P.DVE.reciprocal(e_[:], e_[:], r=[e_], w=[e_])
                    sg_ = sgt.next()
                    P.DVE.tensor_tensor(sg_[:], pk[1][:], e_[:], op=ALU.mult, r=[pk[1], e_], w=[sg_])
                    P.dma(P.SP, out=sgd[r0:r0 + 128, :], in_=sg_[:], r=[sg_])
                    s4 = sm4.next()
                    gu_, gv_ = gu.next(), gv.next()
                    P.ACT.activation(out=gu_[:], in_=pk[2][:], func=AF.Gelu, r=[pk[2]], w=[gu_])
                    P.ACT.activation(out=gv_[:], in_=pk[3][:], func=AF.Gelu, r=[pk[3]], w=[gv_])
                    P.ACT.activation(out=junk[:, 0:512], in_=gv_[:], func=AF.Square, r=[gv_], w=[junk])
                    P.DVE.tensor_reduce(out=s4[:, 0:1], in_=gv_[:], axis=AX.X, op=ALU.add, r=[gv_], w=[s4])
                    P.DVE.tensor_reduce(out=s4[:, 1:2], in_=junk[:, 0:512], axis=AX.X, op=ALU.add, r=[junk], w=[s4])
                    P.DVE.tensor_scalar(s4[:, 2:3], s4[:, 0:1], 1.0 / 512, None, op0=ALU.mult, r=[s4], w=[s4])
                    P.DVE.tensor_tensor(s4[:, 3:4], s4[:, 2:3], s4[:, 2:3], op=ALU.mult, r=[s4], w=[s4])
                    P.DVE.tensor_scalar(s4[:, 4:5], s4[:, 1:2], 1.0 / 512, EPS, op0=ALU.mult, op1=ALU.add, r=[s4], w=[s4])
                    P.DVE.tensor_tensor(s4[:, 4:5], s4[:, 4:5], s4[:, 3:4], op=ALU.subtract, r=[s4], w=[s4])
                    P.ACT.activation(out=s4[:, 4:5], in_=s4[:, 4:5], func=AF.Ln, r=[s4], w=[s4])
                    P.ACT.activation(out=s4[:, 4:5], in_=s4[:, 4:5], func=AF.Exp, scale=-0.5, r=[s4], w=[s4])
                    P.DVE.tensor_scalar(gv_[:], gv_[:], s4[:, 2:3], s4[:, 4:5], op0=ALU.subtract, op1=ALU.mult, r=[gv_, s4], w=[gv_])
                    vn_ = vn.next()
                    P.DVE.tensor_tensor(vn_[:], gv_[:], sgg[:], op=ALU.mult, r=[gv_, sgg], w=[vn_])
                    pss = psSp.next()
                    for g in range(4):
                        P.PE.matmul(pss[:, g * 128:(g + 1) * 128], lhsT=wsT[:, g, :], rhs=vn_[:, g * 128:(g + 1) * 128], start=True, stop=True,
                                    r=[wsT, vn_], w=[pss], inc=(g == 3), skip_group_check=True)
                    P.DVE.tensor_tensor(gv_[:], pss[:], bsb[:], op=ALU.add, r=[pss, bsb], w=[gv_])
                    so_ = so.next()
                    P.DVE.tensor_tensor(so_[:], gv_[:], gu_[:], op=ALU.mult, r=[gv_, gu_], w=[so_])
                    P.dma(P.SP, out=mixcat[r0:r0 + 128, 512:1024], in_=so_[:], r=[so_])
            P.end_phase()
        if dbg.get("mix0_stop") == "A":
            return

        with ExitStack() as st:
            tri = P.tile(st, "tri", [128, 128], BF16)
            tril = P.tile(st, "tril", [128, 128], BF16)
            P.dma(P.POOL, out=tri[:], in_=io["tri"][:, :], w=[tri])
            P.dma(P.POOL, out=tril[:], in_=io["tril"][:, :], w=[tril])
            lbT = P.tile(st, "lbT", [128, 2, 4], F32)
            lb = P.tile(st, "lb", [128, 4], F32)
            omlb = P.tile(st, "omlb", [128, 4], F32)
            P.dma(P.SP, out=lbT[:], in_=io["ev_lbT"][:, :, :], w=[lbT])
            P.DVE.tensor_tensor(lb[:], lbT[:, 1, :], lbT[:, 0, :], op=ALU.subtract, r=[lbT], w=[lb])
            P.ACT.activation(out=lb[:], in_=lb[:], func=AF.Exp, r=[lb], w=[lb])
            P.DVE.tensor_scalar(lb[:], lb[:], 1.0, None, op0=ALU.add, r=[lb], w=[lb])
            P.DVE.reciprocal(lb[:], lb[:], r=[lb], w=[lb])
            P.DVE.tensor_scalar(omlb[:], lb[:], -1.0, 1.0, op0=ALU.mult, op1=ALU.add, r=[lb], w=[omlb])
            ong = P.tile(st, "ong", [128, 128], F32)
            load_bcast(P, ong, io["ev_onorm_g"][0:1, :])
            rmask = P.tile(st, "rmask", [128, T], F32)
            P.DVE.memset(rmask[:], 1.0, w=[rmask])
            P.DVE.memset(rmask[:].rearrange("p (c t) -> p c t", t=128)[:, :, 0:1], 0.0, w=[rmask])
            qT = P.tile(st, "qT", [128, T], F32)
            Aa = P.tile(st, "Aa", [128, T], F32)
            Kk = P.tile(st, "Kk", [128, T], F32)
            Cc = P.tile(st, "Cc", [128, T], F32)
            Ee = P.tile(st, "Ee", [128, T], F32)
            qd = [P.tile(st, "qd", [128, T], BF16) for _ in range(2)]
            kd = [P.tile(st, "kd", [128, T], BF16) for _ in range(2)]
            ktl = [P.tile(st, "ktl", [128, T], BF16) for _ in range(2)]
            ktok = [P.tile(st, "ktok", [128, NCH, 128], BF16) for _ in range(2)]
            Sb = [P.tile(st, "Sb", [128, NCH, 128], BF16) for _ in range(2)]
            dec = [P.tile(st, "dec", [128, NCH], F32) for _ in range(2)]
            bl = P.tile(st, "bl", [128, NCH], F32)
            Sst = P.tile(st, "Sst", [128, 128], F32)
            ivh = P.tile(st, "ivh", [128, NCH, 128], BF16)
            sgh = P.tile(st, "sgh", [128, NCH, 128], BF16)
            gsg = P.tile(st, "gsg", [128, NCH, 128], F32)
            osb = P.tile(st, "osb", [128, NCH, 128], F32)
            hgt = P.tile(st, "hgt", [128, NCH, 128], BF16)
            ssq = P.tile(st, "ssq", [128, NCH], F32)
            osq = P.tile(st, "osq", [128, NCH, 128], F32)
            scT = [Pool(P, st, "scT", [128, 128], BF16, 8) for _ in range(2)]
            psT = Pool(P, st, "psTb0", [128, 8, 128], BF16, 1, psum=True)
            psD = Pool(P, st, "psD", [128, 512], F32, 2, psum=True)
            psS = Pool(P, st, "psS0", [128, 512], F32, 2, psum=True)
            psO = Pool(P, st, "psO0", [128, 512], F32, 2, psum=True)
            c3 = lambda ap: ap.rearrange("p (c t) -> p c t", t=128)
            orders = [list(range(NCH)), [1, 0] + list(range(NCH - 1, 1, -1))]
            for hd in range(4):
                P.dma(P.SP, out=qT[:], in_=fm[hd], w=[qT])
                P.dma(P.SP, out=ivh[:], in_=iv[:, hd * 128:(hd + 1) * 128].rearrange("(c p) v -> p c v", p=128), w=[ivh])
                P.dma(P.SP, out=sgh[:], in_=sgd[:, hd * 128:(hd + 1) * 128].rearrange("(c p) v -> p c v", p=128), w=[sgh])
                P.DVE.tensor_tensor(gsg[:], sgh[:], ong[:].unsqueeze(1).to_broadcast([128, NCH, 128]), op=ALU.mult, r=[sgh, ong], w=[gsg])
                for dr in range(2):
                    P.dma(P.SP, out=Aa[:], in_=fm[4 + 4 * dr + hd], w=[Aa])
                    P.ACT.activation(out=Aa[:], in_=Aa[:], func=AF.Exp, scale=-1.0, r=[Aa], w=[Aa])
                    P.DVE.tensor_scalar(Aa[:], Aa[:], 1.0, None, op0=ALU.add, r=[Aa], w=[Aa])
                    P.DVE.reciprocal(Aa[:], Aa[:], r=[Aa], w=[Aa])
                    P.DVE.tensor_scalar(Aa[:], Aa[:], omlb[:, hd:hd + 1], lb[:, hd:hd + 1], op0=ALU.mult, op1=ALU.add, r=[Aa, omlb, lb], w=[Aa])
                    P.DVE.tensor_scalar(Kk[:], Aa[:], -1.0, 1.0, op0=ALU.mult, op1=ALU.add, r=[Aa], w=[Kk])
                    P.ACT.activation(out=Aa[:], in_=Aa[:], func=AF.Ln, r=[Aa], w=[Aa])
                    P.DVE.tensor_tensor_scan(out=Cc[:], data0=rmask[:], data1=Aa[:], initial=0.0, op0=ALU.mult, op1=ALU.add, r=[rmask, Aa], w=[Cc])
                    P.DVE.tensor_copy(bl[:], c3(Cc[:])[:, :, 127], r=[Cc], w=[bl])
                    if dr == 1:
                        P.DVE.tensor_tensor(c3(Cc[:]), bl[:].unsqueeze(2).to_broadcast([128, NCH, 128]), c3(Cc[:]), op=ALU.subtract, r=[bl, Cc], w=[Cc])
                        P.DVE.tensor_tensor(Cc[:], Cc[:], Aa[:], op=ALU.add, r=[Cc, Aa], w=[Cc])
                    P.ACT.activation(out=dec[dr][:], in_=bl[:], func=AF.Exp, r=[bl], w=[dec[dr]])
                    P.ACT.activation(out=Ee[:], in_=Cc[:], func=AF.Exp, r=[Cc], w=[Ee])
                    P.DVE.tensor_tensor(qd[dr][:], qT[:], Ee[:], op=ALU.mult, r=[qT, Ee], w=[qd[dr]])
                    P.ACT.activation(out=Ee[:], in_=Cc[:], func=AF.Exp, scale=-1.0, r=[Cc], w=[Ee])
                    P.DVE.tensor_tensor(kd[dr][:], Kk[:], Ee[:], op=ALU.mult, r=[Kk, Ee], w=[kd[dr]])
                    P.DVE.tensor_tensor(c3(Cc[:]), bl[:].unsqueeze(2).to_broadcast([128, NCH, 128]), c3(Cc[:]), op=ALU.subtract, r=[bl, Cc], w=[Cc])
                    P.ACT.activation(out=Ee[:], in_=Cc[:], func=AF.Exp, r=[Cc], w=[Ee])
                    P.DVE.tensor_tensor(ktl[dr][:], Kk[:], Ee[:], op=ALU.mult, r=[Kk, Ee], w=[ktl[dr]])
                    for n0 in range(0, NCH, 8):
                        nn = min(8, NCH - n0)
                        pt = psT.next()
                        for j in range(nn):
                            P.PE.transpose(pt[:, j, :], ktl[dr][:, (n0 + j) * 128:(n0 + j + 1) * 128], identb[:], r=[ktl[dr], identb], w=[pt], inc=(j == nn - 1))
                        P.ACT.copy(out=ktok[dr][:, n0:n0 + nn, :], in_=pt[:, 0:nn, :], r=[pt], w=[ktok[dr]])
                    P.DVE.memset(Sst[:], 0.0, w=[Sst])
                    order = orders[dr]
                    for g0 in range(0, NCH, 4):
                        grp = order[g0:g0 + 4]
                        pd = psD.next()
                        for j, n in enumerate(grp):
                            P.PE.matmul(pd[:, j * 128:(j + 1) * 128], lhsT=ktok[dr][:, n, :], rhs=ivh[:, n, :], start=True, stop=True,
                                        r=[ktok[dr], ivh], w=[pd], inc=(j == len(grp) - 1), skip_group_check=True)
                        for j, n in enumerate(grp):
                            P.ACT.copy(out=Sb[dr][:, n, :], in_=Sst[:], r=[Sst], w=[Sb[dr]])
                            P.DVE.scalar_tensor_tensor(Sst[:], in0=Sst[:], scalar=dec[dr][:, n:n + 1], in1=pd[:, j * 128:(j + 1) * 128],
                                                       op0=ALU.mult, op1=ALU.add, r=[Sst, dec[dr], pd], w=[Sst])
                for g0 in range(0, NCH, 4):
                    grp = list(range(g0, min(g0 + 4, NCH)))
                    sc_t = {}
                    for dr in range(2):
                        psc = psS.next()
                        for j, n in enumerate(grp):
                            P.PE.matmul(psc[:, j * 128:(j + 1) * 128], lhsT=kd[dr][:, n * 128:(n + 1) * 128], rhs=qd[dr][:, n * 128:(n + 1) * 128],
                                        start=True, stop=True, r=[kd[dr], qd[dr]], w=[psc], inc=(j == len(grp) - 1), skip_group_check=True)
                        for j, n in enumerate(grp):
                            s_ = scT[dr].next()
                            P.DVE.tensor_tensor(s_[:], psc[:, j * 128:(j + 1) * 128], (tri if dr == 0 else tril)[:], op=ALU.mult,
                                                r=[psc, tri, tril], w=[s_])
                            sc_t[(dr, n)] = s_
                    po = psO.next()
                    for j, n in enumerate(grp):
                        o_ap = po[:, j * 128:(j + 1) * 128]
                        P.PE.matmul(o_ap, lhsT=sc_t[(0, n)][:], rhs=ivh[:, n, :], start=(j == 0), stop=False, r=[sc_t[(0, n)], ivh], w=[po], inc=False, skip_group_check=True)
                        P.PE.matmul(o_ap, lhsT=qd[0][:, n * 128:(n + 1) * 128], rhs=Sb[0][:, n, :], start=False, stop=False, r=[qd[0], Sb[0]], w=[po], inc=False, skip_group_check=True)
                        P.PE.matmul(o_ap, lhsT=sc_t[(1, n)][:], rhs=ivh[:, n, :], start=False, stop=False, r=[sc_t[(1, n)], ivh], w=[po], inc=False, skip_group_check=True)
                        P.PE.matmul(o_ap, lhsT=qd[1][:, n * 128:(n + 1) * 128], rhs=Sb[1][:, n, :], start=False, stop=True, r=[qd[1], Sb[1]], w=[po], inc=(j == len(grp) - 1), skip_group_check=True)
                    P.DVE.tensor_copy(osb[:, g0:g0 + len(grp), :], po[:, 0:len(grp) * 128].rearrange("p (c v) -> p c v", v=128), r=[po], w=[osb])
                P.ACT.activation(out=osq[:], in_=osb[:], func=AF.Square, r=[osb], w=[osq])
                P.DVE.tensor_reduce(out=ssq[:], in_=osq[:], axis=AX.X, op=ALU.add, r=[osq], w=[ssq])
                P.DVE.tensor_scalar(ssq[:], ssq[:], 1.0 / 128, EPS, op0=ALU.mult, op1=ALU.add, r=[ssq], w=[ssq])
                P.ACT.activation(out=ssq[:], in_=ssq[:], func=AF.Ln, r=[ssq], w=[ssq])
                P.ACT.activation(out=ssq[:], in_=ssq[:], func=AF.Exp, scale=-0.5, r=[ssq], w=[ssq])
                P.DVE.tensor_tensor(osb[:], osb[:], ssq[:].unsqueeze(2).to_broadcast([128, NCH, 128]), op=ALU.mult, r=[osb, ssq], w=[osb])
                P.DVE.tensor_tensor(hgt[:], osb[:], gsg[:], op=ALU.mult, r=[osb, gsg], w=[hgt])
                P.dma(P.SP, out=mixcat[:, hd * 128:(hd + 1) * 128].rearrange("(c p) v -> p c v", p=128), in_=hgt[:], r=[hgt])
            P.end_phase()
        if dbg.get("mix0_stop") == "B":
            return

        with ExitStack() as st:
            wout = P.tile(st, "wout0", [128, 8, D], BF16)
            for hh in range(2):
                P.dma(P.POOL, out=wout[:, hh * 4:(hh + 1) * 4, :], in_=io["ev_w_out"][hh * 512:(hh + 1) * 512, :].rearrange("(c p) n -> p c n", p=128), w=[wout])
            G1 = [P.tile(st, "G1", [128, D], F32) for _ in range(2)]
            load_bcast(P, G1[0], io["modv"][l, b, 2:3, :])
            load_bcast(P, G1[1], io["modv"][l, 2, 2:3, :])
            mc = Pool(P, st, "mc", [128, D], BF16, 2)
            mT = Pool(P, st, "mT", [128, 8, 128], BF16, 2)
            xp = Pool(P, st, "cx0", [128, D], F32, 2)
            yp = Pool(P, st, "cy0", [128, D], F32, 2)
            psT = Pool(P, st, "psTc", [128, 8, 128], BF16, 2, psum=True)
            psY = Pool(P, st, "psY", [128, 512], F32, 4, psum=True)
            for n in range(NCH):
                isx = n >= 2
                src = xin(n - 2) if isx else cin(n)
                dst = xout(n - 2) if isx else cout(n)
                g1 = G1[0] if isx else G1[1]
                m_ = mc.next()
                P.dma(P.SP, out=m_[:], in_=mixcat[n * 128:(n + 1) * 128, :], w=[m_])
                xt = xp.next()
                P.dma(P.SP, out=xt[:], in_=src, w=[xt])
                pt = psT.next()
                for kc in range(8):
                    P.PE.transpose(pt[:, kc, :], m_[:, kc * 128:(kc + 1) * 128], identb[:], r=[m_, identb], w=[pt], inc=(kc == 7))
                mT_ = mT.next()
                P.ACT.copy(out=mT_[:], in_=pt[:], r=[pt], w=[mT_])
                y = yp.next()
                for hf in range(2):
                    ps = psY.next()
                    for kc in range(8):
                        P.PE.matmul(ps[:], lhsT=mT_[:, kc, :], rhs=wout[:, kc, hf * 512:(hf + 1) * 512], start=(kc == 0), stop=(kc == 7),
                                    r=[mT_, wout], w=[ps], inc=(kc == 7))
                    P.DVE.tensor_tensor(y[:, hf * 512:(hf + 1) * 512], ps[:], g1[:, hf * 512:(hf + 1) * 512], op=ALU.mult, r=[ps, g1], w=[y])
                P.DVE.tensor_tensor(xt[:], xt[:], y[:], op=ALU.add, r=[xt, y], w=[xt])
                P.dma(P.SP, out=dst, in_=xt[:], r=[xt])
            P.end_phase()


WEIGHT_SPECS = [
    ("ada_w", [2, D, 6 * D]), ("ada_b", [2, 6 * D]), ("norm_mix_g", [2, D]), ("norm_ffn_g", [2, D]),
    ("moe_w_gu", [2, NEXP, D, 2 * FF]), ("moe_w_dn", [2, NEXP, FF, D]), ("final_g", [1, D]),
    ("wr", [2, D, 36]), ("rb", [2, 36]),
    ("od_w_in_ext", [D, 896]), ("od_w_ukv_r", [256, 2048]), ("od_w_uq_ext", [384, 2048]), ("od_w_out", [D, D]),
    ("od_gq", [128, 3]), ("od_gkv", [128, 2]), ("ropecos", [128, CTX + SEQ]), ("ropesin", [128, CTX + SEQ]),
    ("ev_w_in", [D, 3584]), ("ev_w_out", [D, D]), ("ev_wsT", [4, 128, 128]), ("ev_bsb", [128, 512]), ("ev_sgu_g", [1, 512]),
    ("ev_lbT", [128, 2, 4]), ("ev_onorm_g", [1, 128]), ("tril", [128, 128]),
    ("ident", [128, 128]), ("tri", [128, 128]), ("ecb", [128, NEXP]), ("trash", [128, 1]),
]


def build_program(phases=("adaln", "moe0"), dbg=None):
    dbg = dict(dbg or {})
    if dbg.pop("pingpong", True) and "moe0_dst" not in dbg:
        dbg["moe0_dst"] = ("xres2", "cres2")
        dbg.setdefault("mix1_src", ("xres2", "cres2"))
    nc = bass.Bass("TRN2", target_bir_lowering=False)
    io = {}

    def dram(name, shape, dtype=F32, kind="Internal"):
        if name in dbg.get("inputs", ()):
            kind = "ExternalInput"
        if name in dbg.get("outputs", ()) or (kind == "Internal" and dbg.get("no_internal")):
            kind = "ExternalOutput"
        io[name] = nc.dram_tensor(name, list(shape), dtype, kind=kind).ap()
        return io[name]

    dram("x", [BPC * SEQ, D], kind="ExternalInput")
    dram("ctx", [BPC * CTX, D], kind="ExternalInput")
    dram("ccT", [128, 8, 3], kind="ExternalInput")
    for name, shape in WEIGHT_SPECS:
        if name in dbg.get("skip_weights", ()):
            continue
        dram(name, shape, kind="ExternalInput")
    dram("out", [BPC * SEQ, D], kind="ExternalOutput")
    dram("modv", [2, 3, 6, D])
    dram("xres", [BPC * SEQ, D])
    dram("cres", [BPC * CTX, D])
    dram("xs", [NEXP * CAP + 128, D], BF16)
    dram("ys", [NEXP * CAP + 128, D])

    def snap(P, tag):
        if tag not in dbg.get("snap", ()):
            return
        names_ = ("xres", "cres") if tag == "mix0" else dbg.get("mix1_src", ("xres", "cres"))
        for src_, base_ in zip(names_, ("xres", "cres")):
            dst_ = "%s_%s" % (base_, tag)
            dram(dst_, io[src_].shape, kind="ExternalOutput")
            with ExitStack() as st:
                cp = Pool(P, st, "cp", [128, D], F32, 2)
                for i in range(io[src_].shape[0] // 128):
                    t_ = cp.next()
                    P.dma(P.SP, out=t_[:], in_=io[src_][i * 128:(i + 1) * 128, :], w=[t_])
                    P.dma(P.SP, out=io[dst_][i * 128:(i + 1) * 128, :], in_=t_[:], r=[t_])
                P.end_phase()

    with ExitStack() as gstack:
        P = Prog(nc, gstack)
        if "adaln" in phases:
            phase_adaln(P, io)
        fused = dbg.get("fused", False)
        xt_tiles = lambda src, dst: [(io[src][b * SEQ + i * 128: b * SEQ + (i + 1) * 128, :],
                                      io[dst][b * SEQ + i * 128: b * SEQ + (i + 1) * 128, :], b)
                                     for b in range(BPC) for i in range(SEQ // 128)]
        ct_tiles = lambda src, dst: [(io[src][b * CTX + i * 128: b * CTX + (i + 1) * 128, :],
                                      io[dst][b * CTX + i * 128: b * CTX + (i + 1) * 128, :], 2)
                                     for b in range(BPC) for i in range(CTX // 128)]
        if "mix0" in phases:
            dram("fm", [12, 128, CTX + SEQ])
            dram("iv", [CTX + SEQ, 512], BF16)
            dram("sgd", [CTX + SEQ, 512], BF16)
            dram("mixcat", [CTX + SEQ, D], BF16)
            for b in range(BPC):
                phase_mix0(P, io, 0, b, dbg)
            snap(P, "mix0")
        if "moe0" in phases:
            if fused:
                P.fresh_slots()
            xd, cd = dbg.get("moe0_dst", ("xres", "cres"))
            for nm_ in (xd, cd):
                if nm_ not in io:
                    dram(nm_, io["xres" if nm_ == xd else "cres"].shape)
            if dbg.get("dbg_idx"):
                dram("dbg_idx", [128, 36, 2], I32, kind="ExternalOutput")
                dram("dbg_w", [128, 36, 2], F32, kind="ExternalOutput")
                dram("dbg_sm", [36, 128, 200], F32, kind="ExternalOutput")
            phase_moe(P, io, 0, xt_tiles("xres", xd) + ct_tiles("cres", cd), final=False, dbg=dbg)
            snap(P, "moe0")
        if "mix1" in phases:
            if fused:
                P.fresh_slots()
            for b in range(BPC):
                phase_mla(P, io, 1, b, dbg)
            snap(P, "mix1")
        if "moe1" in phases:
            if fused:
                P.fresh_slots()
            phase_moe(P, io, 1, xt_tiles(dbg.get("mix1_src", ("xres", "cres"))[0], "out"), final=True)
        for src_, dst_ in (dbg.get("alias_out") or {}).items():
            dram(dst_, io[src_].shape, kind="ExternalOutput")
            with ExitStack() as st:
                cp = Pool(P, st, "cp", [128, io[src_].shape[1]], F32, 2)
                for i in range(io[src_].shape[0] // 128):
                    t_ = cp.next()
                    P.dma(P.SP, out=t_[:], in_=io[src_][i * 128:(i + 1) * 128, :], w=[t_])
                    P.dma(P.SP, out=io[dst_][i * 128:(i + 1) * 128, :], in_=t_[:], r=[t_])
                P.end_phase()
        P.barrier()
    return nc


def host_consts():
    ident = np.eye(128, dtype=np.float32)
    tri = np.triu(np.ones((128, 128), np.float32))
    ecb = np.tile((np.arange(NEXP, dtype=np.float32) * CAP - 1.0)[None, :], (128, 1))
    trash = (NEXP * CAP + np.arange(128, dtype=np.float32)).reshape(128, 1)
    inv = (10000.0 ** (-np.arange(0, 32, 2, dtype=np.float32) / 32.0)).astype(np.float32)
    t = np.arange(SEQ, dtype=np.float32)
    ang_r = np.floor(t / 64.0)[:, None] * inv[None, :]
    ang_c = (t % 64.0)[:, None] * inv[None, :]
    cos64 = np.concatenate([np.cos(ang_r), np.cos(ang_r), np.cos(ang_c), np.cos(ang_c)], axis=1).T
    sin64 = np.concatenate([-np.sin(ang_r), np.sin(ang_r), -np.sin(ang_c), np.sin(ang_c)], axis=1).T
    cosf = np.concatenate([np.ones((64, CTX), np.float32), cos64.astype(np.float32)], axis=1)
    sinf = np.concatenate([np.zeros((64, CTX), np.float32), sin64.astype(np.float32)], axis=1)
    ropecos = np.ascontiguousarray(np.concatenate([cosf, cosf], axis=0).astype(np.float32))
    ropesin = np.ascontiguousarray(np.concatenate([sinf, sinf], axis=0).astype(np.float32))
    return dict(ident=ident, tri=tri, tril=np.ascontiguousarray(tri.T), ecb=ecb, trash=trash, ropecos=ropecos, ropesin=ropesin)


def make_in_maps(inputs):
    f = lambda a: np.ascontiguousarray(np.asarray(a, dtype=np.float32))
    x, c, ctx, c_ctx = f(inputs["x"]), f(inputs["c"]), f(inputs["ctx"]), f(inputs["c_ctx"])
    shared = {k: f(inputs[k]) for k in ("ada_w", "ada_b", "norm_mix_g", "norm_ffn_g", "moe_w_gu", "moe_w_dn")}
    shared["final_g"] = f(inputs["final_g"]).reshape(1, D)
    w_r1, w_r2 = f(inputs["moe_w_r1"]), f(inputs["moe_w_r2"])
    shared["wr"] = np.ascontiguousarray(np.concatenate([w_r1, w_r2.transpose(0, 2, 1, 3).reshape(2, D, 32)], axis=2))
    shared["rb"] = np.ascontiguousarray(np.concatenate([f(inputs["moe_b_r1"]), f(inputs["moe_b_r2"]).reshape(2, 32)], axis=1))
    w_in = f(inputs["od_w_in"])[0]
    rope = w_in[:, 640:704]
    swp = lambda r: np.concatenate([r[..., 16:32], r[..., 0:16], r[..., 48:64], r[..., 32:48]], axis=-1)
    shared["od_w_in_ext"] = np.ascontiguousarray(np.concatenate([w_in[:, 0:640], rope, rope, swp(rope), swp(rope)], axis=1))
    w_ukv = f(inputs["od_w_ukv"])[0].reshape(256, 8, 256)
    shared["od_w_ukv_r"] = np.ascontiguousarray(np.concatenate([w_ukv[:, :, 0:128].reshape(256, 1024), w_ukv[:, :, 128:256].reshape(256, 1024)], axis=1))
    w_uq = f(inputs["od_w_uq"])[0].reshape(384, 8, 192)
    shared["od_w_uq_ext"] = np.ascontiguousarray(np.concatenate([w_uq[:, :, 0:128].reshape(384, 1024), w_uq[:, :, 128:192].reshape(384, 512),
                                                                 swp(w_uq[:, :, 128:192]).reshape(384, 512)], axis=1))
    shared["od_w_out"] = f(inputs["od_w_out"])[0]
    shared["od_gq"] = np.ascontiguousarray(f(inputs["od_q_norm_g"])[0].reshape(3, 128).T)
    shared["od_gkv"] = np.ascontiguousarray(f(inputs["od_kv_norm_g"])[0].reshape(2, 128).T)
    shared["ev_w_in"] = f(inputs["ev_w_in"])[0]
    shared["ev_w_out"] = f(inputs["ev_w_out"])[0]
    shared["ev_wsT"] = np.ascontiguousarray(f(inputs["ev_w_s"])[0].transpose(0, 2, 1))
    shared["ev_bsb"] = np.ascontiguousarray(np.repeat(f(inputs["ev_b_s"])[0].T[:, :, None], 128, axis=2).reshape(128, 512))
    shared["ev_sgu_g"] = f(inputs["ev_sgu_g"])[0].reshape(1, 512)
    shared["ev_lbT"] = np.ascontiguousarray(f(inputs["ev_lb_logits"]).reshape(2, 4, 128).transpose(2, 0, 1))
    shared["ev_onorm_g"] = f(inputs["ev_onorm_g"])[0].reshape(1, 128)
    shared.update(host_consts())
    maps = []
    for i in range(NCORES):
        cc = np.stack([c[2 * i], c[2 * i + 1], c_ctx], axis=1)
        ccT = np.ascontiguousarray(cc.reshape(8, 128, 3).transpose(1, 0, 2))
        m = dict(shared)
        m["x"] = np.ascontiguousarray(x[2 * i:2 * i + 2].reshape(BPC * SEQ, D))
        m["ctx"] = np.ascontiguousarray(ctx[2 * i:2 * i + 2].reshape(BPC * CTX, D))
        m["ccT"] = ccT
        maps.append(m)
    return maps


def _launch(phases, dbg, maps, extra=None, drop=()):
    nc = build_program(phases=phases, dbg=dbg)
    in_maps = []
    for i, m in enumerate(maps):
        mm = {k: v for k, v in m.items() if k not in drop}
        if extra:
            mm.update({k: v[i] for k, v in extra.items()})
        in_maps.append(mm)
    res = run_bass_kernel_spmd(nc, in_maps, core_ids=list(range(NCORES)))
    return res.results


MOE_W = ("moe_w_gu", "moe_w_dn")


def kernel(**inputs):
    maps = make_in_maps(inputs)
    r = _launch(("adaln", "mix0", "moe0", "mix1", "moe1"), dict(fused=True), maps)
    out = np.stack([x["out"].reshape(BPC, SEQ, D) for x in r]).reshape(NCORES * BPC, SEQ, D)
    return out.astype(np.float32)
```
